# Optimizing a Trainium2 kernel written in Bass

```python
import jax, jax.numpy as jnp
from jax import lax
import numpy as np

D_MODEL = 1024
BATCH = 8
SEQ = 4096
DEPTH = 2

CHUNK = 64
CONV_WIDTH = 512
CONV_KERNEL = 31
POOL_WINDOWS = (2, 4, 8, 16)
POOL_GROUP = 128
POOL_WIDTH = POOL_GROUP * len(POOL_WINDOWS)
SGU_HEADS = 4
SGU_HEAD_DIM = 128
SGU_WIDTH = SGU_HEADS * SGU_HEAD_DIM
SGU_BLOCK = 128
N_BRANCH = 3
IN_WIDTH = 2 * CONV_WIDTH + POOL_WIDTH + 2 * SGU_WIDTH
D_FF = 2816
N_EXPERTS = 8
TOP_K = 2
D_FF_EXPERT = 3584
N_DENSE = (DEPTH + 1) // 2
N_MOE = DEPTH // 2
EPS = 1e-6

kernel_name = "hybrid_gated_conv_pool_sgu_moe_trunk"


def rmsnorm(x, g):
    xf = x.astype(jnp.float32)
    y = xf * lax.rsqrt(jnp.mean(xf * xf, axis=-1, keepdims=True) + EPS)
    return (y * g.astype(jnp.float32)).astype(x.dtype)


def layernorm(x, g, b):
    xf = x.astype(jnp.float32)
    mu = jnp.mean(xf, axis=-1, keepdims=True)
    var = jnp.mean(jnp.square(xf - mu), axis=-1, keepdims=True)
    y = (xf - mu) * lax.rsqrt(var + EPS)
    return (y * g.astype(jnp.float32) + b.astype(jnp.float32)).astype(x.dtype)


def conv_branch(z, w_dw, b_dw, ln_g, ln_b, w_out):
    a, gate = jnp.split(z, 2, axis=-1)
    h = a * jax.nn.sigmoid(gate)
    h = lax.conv_general_dilated(
        h, w_dw[:, None, :].astype(h.dtype), window_strides=(1,),
        padding=((CONV_KERNEL - 1, 0),),
        dimension_numbers=("NWC", "WIO", "NWC"),
        feature_group_count=CONV_WIDTH) + b_dw
    h = jax.nn.silu(layernorm(h, ln_g, ln_b))
    return h @ w_out


def pool_branch(p, w_mix, scale, w_out):
    B, S, _ = p.shape
    pf = p.astype(jnp.float32)
    csum = jnp.cumsum(pf, axis=1)
    t = jnp.arange(S)
    outs = []
    for g, w in enumerate(POOL_WINDOWS):
        sl = slice(g * POOL_GROUP, (g + 1) * POOL_GROUP)
        cg = csum[..., sl]
        lagged = jnp.pad(cg, ((0, 0), (w, 0), (0, 0)))[:, :S]
        cnt = jnp.minimum(t + 1, w).astype(jnp.float32)[None, :, None]
        outs.append((cg - lagged) / cnt - pf[..., sl])
    d = jnp.stack(outs, axis=2).astype(p.dtype)
    y = jnp.einsum("bsgc,gcd->bsgd", d, w_mix).reshape(B, S, POOL_WIDTH) * scale
    return y @ w_out


def sgu_branch(z, ln_g, ln_b, w_s, b_s, w_out):
    B, S, _ = z.shape
    z = jax.nn.gelu(z)
    u, v = jnp.split(z, 2, axis=-1)
    v = layernorm(v, ln_g, ln_b).reshape(B, S // SGU_BLOCK, SGU_BLOCK, SGU_HEADS, SGU_HEAD_DIM)
    mask = jnp.tril(jnp.ones((SGU_BLOCK, SGU_BLOCK), dtype=bool))
    ws = jnp.where(mask[None], w_s, jnp.zeros_like(w_s))
    sv = jnp.einsum("hqp,bnphd->bnqhd", ws, v) + b_s.T[None, None, :, :, None]
    y = u * sv.reshape(B, S, SGU_WIDTH)
    return y @ w_out


def dense_swiglu(h, w_up, w_down):
    a, b = jnp.split(h @ w_up, 2, axis=-1)
    return (jax.nn.silu(a) * b) @ w_down


def moe_swiglu(h, w_router, w_up, w_down):
    B, S, D = h.shape
    hf = h.reshape(B * S, D)
    logits = (hf @ w_router).astype(jnp.float32)
    top_v, top_i = lax.top_k(logits, TOP_K)
    wts = jax.nn.softmax(top_v, axis=-1)
    gate = jnp.sum(jax.nn.one_hot(top_i, N_EXPERTS, dtype=jnp.float32) * wts[..., None], axis=1)
    out = jnp.zeros((B * S, D), jnp.float32)
    for e in range(N_EXPERTS):
        a, b = jnp.split(hf @ w_up[e], 2, axis=-1)
        out = out + gate[:, e:e + 1] * ((jax.nn.silu(a) * b) @ w_down[e]).astype(jnp.float32)
    return out.astype(h.dtype).reshape(B, S, D)


def setup_inputs(seed: int = 0) -> dict:
    key = jax.random.key(seed)
    ks = jax.random.split(key, 32)
    f = jnp.float32
    D = D_MODEL

    def nrm(k, shape, fan_in):
        return jax.random.normal(k, shape, f) * (fan_in ** -0.5)

    def gain(k, shape):
        return 1.0 + 0.05 * jax.random.normal(k, shape, f)

    def bias(k, shape):
        return 0.02 * jax.random.normal(k, shape, f)

    return {
        "x": jax.random.normal(ks[0], (BATCH, SEQ, D), f),
        "norm_mix": gain(ks[1], (DEPTH, D)),
        "w_in": nrm(ks[2], (DEPTH, D, IN_WIDTH), D),
        "b_in": bias(ks[3], (DEPTH, IN_WIDTH)),
        "conv_w": nrm(ks[4], (DEPTH, CONV_KERNEL, CONV_WIDTH), CONV_KERNEL),
        "conv_b": bias(ks[5], (DEPTH, CONV_WIDTH)),
        "conv_ln_g": gain(ks[6], (DEPTH, CONV_WIDTH)),
        "conv_ln_b": bias(ks[7], (DEPTH, CONV_WIDTH)),
        "conv_out": nrm(ks[8], (DEPTH, CONV_WIDTH, D), CONV_WIDTH),
        "pool_mix": nrm(ks[9], (DEPTH, len(POOL_WINDOWS), POOL_GROUP, POOL_GROUP), POOL_GROUP),
        "pool_scale": gain(ks[10], (DEPTH, POOL_WIDTH)),
        "pool_out": nrm(ks[11], (DEPTH, POOL_WIDTH, D), POOL_WIDTH),
        "sgu_ln_g": gain(ks[12], (DEPTH, SGU_WIDTH)),
        "sgu_ln_b": bias(ks[13], (DEPTH, SGU_WIDTH)),
        "sgu_w": nrm(ks[14], (DEPTH, SGU_HEADS, SGU_BLOCK, SGU_BLOCK), SGU_BLOCK),
        "sgu_b": gain(ks[15], (DEPTH, SGU_HEADS, SGU_BLOCK)),
        "sgu_out": nrm(ks[16], (DEPTH, SGU_WIDTH, D), SGU_WIDTH),
        "w_gate": nrm(ks[17], (DEPTH, D, N_BRANCH * D), D),
        "b_gate": bias(ks[18], (DEPTH, N_BRANCH * D)),
        "w_o": nrm(ks[19], (DEPTH, D, D), D),
        "norm_ffn": gain(ks[20], (DEPTH, D)),
        "ffn_w_up": nrm(ks[21], (N_DENSE, D, 2 * D_FF), D),
        "ffn_w_down": nrm(ks[22], (N_DENSE, D_FF, D), D_FF),
        "moe_router": nrm(ks[23], (N_MOE, D, N_EXPERTS), D),
        "moe_w_up": nrm(ks[24], (N_MOE, N_EXPERTS, D, 2 * D_FF_EXPERT), D),
        "moe_w_down": nrm(ks[25], (N_MOE, N_EXPERTS, D_FF_EXPERT, D), D_FF_EXPERT),
        "norm_final": gain(ks[26], (D,)),
    }


def reference(x, norm_mix, w_in, b_in, conv_w, conv_b, conv_ln_g, conv_ln_b, conv_out,
              pool_mix, pool_scale, pool_out, sgu_ln_g, sgu_ln_b, sgu_w, sgu_b, sgu_out,
              w_gate, b_gate, w_o, norm_ffn, ffn_w_up, ffn_w_down,
              moe_router, moe_w_up, moe_w_down, norm_final):
    B, S, D = x.shape
    c1 = 2 * CONV_WIDTH
    c2 = c1 + POOL_WIDTH
    for i in range(DEPTH):
        h = rmsnorm(x, norm_mix[i])
        z = h @ w_in[i] + b_in[i]
        z_conv, z_pool, z_sgu = z[..., :c1], z[..., c1:c2], z[..., c2:]
        y_a = conv_branch(z_conv, conv_w[i], conv_b[i], conv_ln_g[i], conv_ln_b[i], conv_out[i])
        y_b = pool_branch(z_pool, pool_mix[i], pool_scale[i], pool_out[i])
        y_c = sgu_branch(z_sgu, sgu_ln_g[i], sgu_ln_b[i], sgu_w[i], sgu_b[i], sgu_out[i])
        g = jax.nn.sigmoid(h @ w_gate[i] + b_gate[i]).reshape(B, S, N_BRANCH, D)
        merged = g[:, :, 0] * y_a + g[:, :, 1] * y_b + g[:, :, 2] * y_c
        x = x + merged @ w_o[i]
        h = rmsnorm(x, norm_ffn[i])
        if i % 2 == 0:
            x = x + dense_swiglu(h, ffn_w_up[i // 2], ffn_w_down[i // 2])
        else:
            x = x + moe_swiglu(h, moe_router[i // 2], moe_w_up[i // 2], moe_w_down[i // 2])
    return rmsnorm(x, norm_final)
```

```python
import contextlib
import numpy as np
import concourse.bass as bass
import concourse.mybir as mybir
from concourse.bass_utils import run_bass_kernel_spmd

F32 = mybir.dt.float32
BF16 = mybir.dt.bfloat16
AF = mybir.ActivationFunctionType
ALU = mybir.AluOpType
AX = mybir.AxisListType

D = 1024
T = 512
DEPTH = 2
CW = 512
CK = 31
HALO_C = CK - 1
POOL_W = (2, 4, 8, 16)
HALO_P = 15
D_FF = 2816
NE = 8
D_FFE = 3584
EPS = 1e-6
CHUNK = 8192
NSLOT = 4
NARENA = 12
GELU_FUNC = "native"


class Buf:
    __slots__ = ("w", "r", "name")

    def __init__(self, name=""):
        self.w = {}
        self.r = {}
        self.name = name


class Item:
    __slots__ = ("name", "R", "W", "pieces", "chunk", "off", "group")

    def __init__(self, name, R, W, pieces, group):
        self.name, self.R, self.W, self.pieces, self.group = name, R, W, pieces, group
        self.chunk = None
        self.off = None


class Builder:
    def __init__(self, nt, seq_total, nexp=NE, upto="all"):
        self.nt = nt
        self.S = nt * T
        self.nexp = nexp
        self.upto = upto
        self.nc = bass.Bass("TRN2", target_bir_lowering=False)
        self.planning = False
        self.items = []
        self.item_by_name = {}
        self.item_pos = 0
        self.cur_group = None
        self.ps_next = 0
        self.ar_next = 0
        self.sm_next = 0

    def setup_tracker(self, stack):
        nc = self.nc
        self.eng = {"pe": nc.tensor, "act": nc.scalar, "dve": nc.vector, "pool": nc.gpsimd, "sp": nc.sync}
        self.sems = {}
        self.cnt = {}
        self.seen = {e: {} for e in self.eng}
        self.stack = stack
        for e in self.eng:
            self.newsem(e)

    def newsem(self, key):
        self.sems[key] = self.stack.enter_context(self.nc.semaphore("s_" + key))
        self.cnt[key] = 0

    def _waits(self, e, reads, writes, extra=()):
        deps = {}

        def add(tok):
            if tok is None:
                return
            k, v = tok
            if deps.get(k, 0) < v:
                deps[k] = v
        for b in reads:
            for k, v in b.w.items():
                add((k, v))
        for b in writes:
            for k, v in b.w.items():
                add((k, v))
            for k, v in b.r.items():
                add((k, v))
        for tok in extra:
            add(tok)
        for k, v in deps.items():
            if k == e:
                if e == "pe" or e == "sp":
                    continue
            if self.seen[e].get(k, 0) >= v:
                continue
            self.eng[e].wait_ge(self.sems[k], v)
            self.seen[e][k] = v

    def op(self, e, fn, reads=(), writes=(), signal=True):
        if self.planning:
            return None
        if self.CONST in reads:
            reads = list(reads) + [self.CONSTP, self.CONSTQ, self.CONSTC]
        self._waits(e, reads, writes)
        ins = fn()
        tok = (e, self.cnt[e] + 1)
        if signal:
            ins.then_inc(self.sems[e], 1)
            self.cnt[e] += 1
        for b in writes:
            b.w = {tok[0]: tok[1]}
            b.r = {}
        for b in reads:
            if b.r.get(e, 0) < tok[1]:
                b.r[e] = tok[1]
        return ins

    def dma(self, q, out, in_, key, reads=(), writes=(), serialize=True, slow=False, accum=False, extra_toks=()):
        if self.planning:
            return
        extra = [(key, self.cnt[key])] if (serialize and self.cnt[key] > 0) else []
        extra = extra + list(extra_toks)
        self._waits(q, reads, writes if not accum else (), extra)
        self.cnt[key] += 16
        tok = (key, self.cnt[key])
        if slow:
            self.eng[q].dma_start(out=out, in_=in_, allow_slow_non_contiguous=True).then_inc(self.sems[key], 16)
        else:
            self.eng[q].dma_start(out=out, in_=in_).then_inc(self.sems[key], 16)
        for b in writes:
            if accum:
                b.w[key] = tok[1]
            else:
                b.w = {key: tok[1]}
                b.r = {}
        for b in reads:
            if b.r.get(key, 0) < tok[1]:
                b.r[key] = tok[1]

    def item(self, name, R=None, W=None, pieces=None):
        if self.planning:
            it = Item(name, R, W, pieces, self.cur_group)
            self.items.append(it)
            self.item_by_name[name] = it
            return it
        it = self.item_by_name[name]
        gchunk = self.tile_idx * self.nchunk + it.chunk
        assert gchunk >= self.ring_cur, (name, gchunk, self.ring_cur)
        if gchunk > self.ring_cur or self.ring_issued == 0:
            self.ring_cur = gchunk
            lim = min(gchunk - 1 + NSLOT, self.nt * self.nchunk - 1)
            while self.ring_issued <= lim:
                g = self.ring_issued
                ch = g % self.nchunk
                slot = g % NSLOT
                used = self.chunk_used[ch]
                sbuf_ = self.slotbuf[slot]
                if g < self.nchunk:
                    pre = list(sbuf_.w.items()) + list(sbuf_.r.items())
                    first = True
                    for cit in self.chunk_items[ch]:
                        sz = cit.R * cit.W
                        if cit.pieces is None:
                            self.dma("sp", self.RING[:, slot, cit.off:cit.off + sz], self.stream[ch, :, cit.off:cit.off + sz],
                                     "slot%d" % slot, reads=[self.hbuf[cit.group]], writes=[sbuf_], accum=not first, extra_toks=pre)
                            first = False
                        else:
                            view = self.RING[:, slot, cit.off:cit.off + sz].rearrange("p (r w) -> p r w", w=cit.W)
                            for (r0, nr, c0, ncw, src) in cit.pieces:
                                self.dma("pool", view[:, r0:r0 + nr, c0:c0 + ncw], src, "slotc%d" % slot, writes=[sbuf_],
                                         serialize=False, accum=not first, extra_toks=pre)
                                first = False
                    for cit in self.chunk_items[ch]:
                        if cit.pieces is not None:
                            sz = cit.R * cit.W
                            self.dma("sp", self.stream[ch, :, cit.off:cit.off + sz], self.RING[:, slot, cit.off:cit.off + sz],
                                     "wb%d" % slot, reads=[sbuf_], writes=[self.wbbuf[ch]], serialize=False, accum=True)
                else:
                    self.dma("sp", self.RING[:, slot, 0:used], self.stream[ch, :, 0:used], "slot%d" % slot,
                             reads=[self.wbbuf[ch]], writes=[sbuf_])
                self.ring_issued += 1
        return it

    def iap(self, it, r, c0, n):
        gchunk = self.tile_idx * self.nchunk + it.chunk
        assert gchunk >= self.ring_cur, ("stale ring item", it.name)
        slot = gchunk % NSLOT
        o = it.off + r * it.W + c0
        return self.RING[:, slot, o:o + n]

    def ibuf(self, it):
        gchunk = self.tile_idx * self.nchunk + it.chunk
        return self.slotbuf[gchunk % NSLOT]

    def pack_items(self):
        ch, off = 0, 0
        self.chunk_groups = {}
        self.chunk_items = {}
        self.chunk_used = {}
        for it in self.items:
            sz = it.R * it.W
            assert sz <= CHUNK
            if off + sz > CHUNK:
                ch += 1
                off = 0
            it.chunk, it.off = ch, off
            off += sz
            self.chunk_groups.setdefault(ch, set()).add(it.group)
            self.chunk_items.setdefault(ch, []).append(it)
            self.chunk_used[ch] = off
        self.nchunk = ch + 1

    def psum(self):
        i = self.ps_next
        self.ps_next = (i + 1) % 8
        return i

    def arena(self, n=1):
        i = self.ar_next
        if i + n > NARENA:
            i = 0
        self.ar_next = (i + n) % NARENA
        return i

    def mm_group(self, bank_ap, bankbuf, specs, extra_reads=()):
        n = len(specs)
        for i, (lf, rf, rb) in enumerate(specs):
            self.op("pe", (lambda lf=lf, rf=rf, i=i: self.nc.tensor.matmul(bank_ap(), lf(), rf(), start=(i == 0), stop=(i == n - 1))),
                    reads=list(rb) + list(extra_reads), writes=[bankbuf], signal=(i == n - 1))

    def build(self):
        nc = self.nc
        S = self.S
        dt_in = {}

        def din(name, shape):
            dt_in[name] = nc.dram_tensor(name, list(shape), F32, kind="ExternalInput").ap()
            return dt_in[name]
        self.x_d = din("x", (S, D))
        self.norm_mix = din("norm_mix", (DEPTH, D))
        self.w_in = din("w_in", (DEPTH, D, 2560))
        self.b_in = din("b_in", (DEPTH, 2560))
        self.conv_w = din("conv_w", (DEPTH, CK, CW))
        self.conv_b = din("conv_b", (DEPTH, CW))
        self.conv_ln_g = din("conv_ln_g", (DEPTH, CW))
        self.conv_ln_b = din("conv_ln_b", (DEPTH, CW))
        self.conv_out = din("conv_out", (DEPTH, CW, D))
        self.pool_mix = din("pool_mix", (DEPTH, 4, 128, 128))
        self.pool_scale = din("pool_scale", (DEPTH, 512))
        self.pool_out = din("pool_out", (DEPTH, 512, D))
        self.sgu_ln_g = din("sgu_ln_g", (DEPTH, 512))
        self.sgu_ln_b = din("sgu_ln_b", (DEPTH, 512))
        self.sgu_w = din("sgu_w", (DEPTH, 4, 128, 128))
        self.sgu_b = din("sgu_b", (DEPTH, 4, 128))
        self.sgu_out = din("sgu_out", (DEPTH, 512, D))
        self.w_gate = din("w_gate", (DEPTH, D, 3 * D))
        self.b_gate = din("b_gate", (DEPTH, 3 * D))
        self.w_o = din("w_o", (DEPTH, D, D))
        self.norm_ffn = din("norm_ffn", (DEPTH, D))
        self.ffn_w_up = din("ffn_w_up", (1, D, 2 * D_FF))
        self.ffn_w_down = din("ffn_w_down", (1, D_FF, D))
        self.moe_router = din("moe_router", (1, D, NE))
        self.moe_w_up = din("moe_w_up", (1, NE, D, 2 * D_FFE))
        self.moe_w_down = din("moe_w_down", (1, NE, D_FFE, D))
        self.norm_final = din("norm_final", (D,))
        self.out_d = nc.dram_tensor("out", [S, D], F32, kind="ExternalOutput").ap()

        self.planning = True
        self.tile_idx = 0
        self.tile_program(0)
        self.planning = False
        self.pack_items()
        self.stream = nc.dram_tensor("wstream", [self.nchunk, 128, CHUNK], BF16).ap()

        with contextlib.ExitStack() as st:
            self.setup_tracker(st)

            def sb(name, shape, dt):
                return st.enter_context(nc.sbuf_tensor(name, list(shape), dt))
            self.X = sb("X", (128, 4, 2, 512), F32)
            self.XB = [[Buf("x%d%d" % (tb, dh)) for dh in range(2)] for tb in range(4)]
            self.HT = sb("HT", (128, 8, 512), BF16)
            self.HTB = [Buf("hT%d" % tb) for tb in range(4)]
            self.HC = [sb("HC%d" % l, (128, 4, HALO_C + T), BF16) for l in range(DEPTH)]
            self.HCB = [[Buf() for c in range(4)] for l in range(DEPTH)]
            self.HCH = [[Buf() for c in range(4)] for l in range(DEPTH)]
            self.PP = [sb("PP%d" % l, (128, 4, HALO_P + T), F32) for l in range(DEPTH)]
            self.PPB = [[Buf() for c in range(4)] for l in range(DEPTH)]
            self.PPH = [[Buf() for c in range(4)] for l in range(DEPTH)]
            self.PS1 = sb("PS1", (128, HALO_P + T + 1), F32)
            self.PS2 = sb("PS2", (128, HALO_P + T + 1), F32)
            self.PS1B, self.PS2B = Buf(), Buf()
            self.CT = sb("CT", (128, 4, 512), BF16)
            self.CTB = [Buf() for _ in range(4)]
            self.YP = sb("YP", (128, 4, 512), BF16)
            self.YPB = [Buf() for _ in range(4)]
            self.YS = sb("YS", (128, 4, 512), BF16)
            self.YSB = [Buf() for _ in range(4)]
            self.DT, self.DTB = self.YS, self.YSB
            self.HID = sb("HID", (128, 2, 8, 512), BF16)
            self.HIDB = [[Buf() for _ in range(8)] for _ in range(2)]
            self.VL, self.VLB = self.HID[:, 1, 0:4, :], self.HIDB[1][0:4]
            self.UT, self.UTB = self.HID[:, 1, 4:8, :], self.HIDB[1][4:8]
            self.A32 = sb("A32", (128, NARENA, 512), F32)
            self.AB = [Buf("ar%d" % i) for i in range(NARENA)]
            self.RING = sb("RING", (128, NSLOT, CHUNK), BF16)
            self.slotbuf = [Buf("slot%d" % i) for i in range(NSLOT)]
            self.SM = sb("SM", (128, 32, 16), F32)
            self.SMB = [Buf() for _ in range(32)]
            self.sm_next = 0
            self.GATE = sb("GATE", (128, 4, 8), F32)
            self.GATEB = [Buf() for _ in range(4)]
            self.ident = sb("ident", (128, 128), F32)
            self.identb = sb("identb", (128, 128), BF16)
            self.onesm = sb("onesm", (128, 128), F32)
            self.onesrow = sb("onesrow", (1, 128), BF16)
            self.bsrow = sb("bsrow", (1, DEPTH, 4, 512), BF16)
            self.COLS = sb("COLS", (128, DEPTH, 76), F32)
            self.bin_col = self.COLS[:, :, 0:20]
            self.bgate_col = self.COLS[:, :, 20:44]
            self.convb_col = self.COLS[:, :, 44:48]
            self.clng_col = self.COLS[:, :, 48:52]
            self.clnb_col = self.COLS[:, :, 52:56]
            self.pscale_col = self.COLS[:, :, 56:60]
            self.nmix_col = self.COLS[:, :, 60:68]
            self.nffn_col = self.COLS[:, :, 68:76]
            self.cw_col = sb("cw_col", (128, DEPTH, 4, CK), F32)
            self.sgug_bc = sb("sgug_bc", (128, DEPTH, 512), F32)
            self.sgub_bc = sb("sgub_bc", (128, DEPTH, 512), F32)
            self.bv_bc = sb("bv_bc", (128, DEPTH, 512), F32)
            self.nfin_bc = sb("nfin_bc", (128, 2, 512), F32)
            self.wsT = sb("wsT", (128, DEPTH, 4, 128), BF16)
            self.wmix = sb("wmix", (128, DEPTH, 4, 128), BF16)
            self.R32 = sb("R32", (128, 8, NE), F32)
            self.icnt = sb("icnt", (128, 4, 16), F32)
            self.CONST = Buf("const")
            self.CONSTP = Buf("constp")
            self.CONSTQ = Buf("constq")
            self.CONSTC = Buf("constc")
            self.PS = st.enter_context(nc.psum_tensor("PS", [128, 8, 512], F32))
            self.PSB = [Buf("ps%d" % i) for i in range(8)]
            self.ps_next = 0
            self.ar_next = 0
            for i in range(NSLOT):
                self.newsem("slot%d" % i)
                self.newsem("slotc%d" % i)
                self.newsem("wb%d" % i)
            self.wbbuf = {ch: Buf("wb%d" % ch) for ch in range(self.nchunk)}
            for tb in range(4):
                self.newsem("xin%d" % tb)
                self.newsem("out%d" % tb)
            self.newsem("cst")
            self.newsem("cst2")
            self.newsem("cstq")
            self.groups = []
            for it in self.items:
                if it.group not in self.groups:
                    self.groups.append(it.group)
            self.gbuf = {}
            self.hbuf = {}
            for g in self.groups:
                self.newsem("h_" + g)
                self.hbuf[g] = Buf("h_" + g)

            self.prologue()
            self.ring_cur = 0
            self.ring_issued = 0
            self.load_x(0)
            for ti in range(self.nt):
                self.tile_idx = ti
                self.item_pos = 0
                self.tile_program(ti)
            for tb in range(4):
                nc.sync.wait_ge(self.sems["out%d" % tb], self.cnt["out%d" % tb])
        return nc

    def cdma(self, out, in_, slow=True, q="pool"):
        if q == "pool":
            self.dma(q, out, in_, "cstq", writes=[self.CONSTQ], serialize=False, slow=slow)
        else:
            self.dma(q, out, in_, "cst", writes=[self.CONST], serialize=False, slow=slow)

    def prologue(self):
        nc = self.nc
        C = self.CONSTP
        self.op("pool", lambda: nc.gpsimd.memset(self.ident[:], 1.0), writes=[C])
        self.op("pool", lambda: nc.gpsimd.affine_select(out=self.ident[:], in_=self.ident[:], pattern=[[1, 128]],
                                                        compare_op=ALU.is_equal, fill=0.0, base=0, channel_multiplier=-1),
                reads=[C], writes=[C])
        self.op("pool", lambda: nc.gpsimd.tensor_copy(out=self.identb[:], in_=self.ident[:]), reads=[C], writes=[C])
        self.op("pool", lambda: nc.gpsimd.memset(self.onesm[:], 1.0 / CW), writes=[C])
        self.op("pool", lambda: nc.gpsimd.memset(self.onesrow[:], 1.0), writes=[C])
        for l in range(DEPTH):
            self.op("pool", lambda l=l: nc.gpsimd.memset(self.HC[l][:, :, 0:HALO_C], 0.0), writes=[b for b in self.HCH[l]])
            self.op("pool", lambda l=l: nc.gpsimd.memset(self.PP[l][:, :, 0:HALO_P], 0.0), writes=[b for b in self.PPH[l]])
        for g, w in enumerate(POOL_W):
            self.op("pool", lambda g=g, w=w: nc.gpsimd.memset(self.icnt[:, g, :], 1.0 / w), writes=[C])
            for t in range(w - 1):
                self.op("pool", lambda g=g, t=t: nc.gpsimd.memset(self.icnt[:, g, t:t + 1], 1.0 / (t + 1)), writes=[C])

        specs = [(self.b_in, 20), (self.b_gate, 24), (self.conv_b, 4), (self.conv_ln_g, 4), (self.conv_ln_b, 4),
                 (self.pool_scale, 4), (self.norm_mix, 8), (self.norm_ffn, 8)]
        stg, stw = [], []
        for l in range(DEPTH):
            a = self.arena(1)
            stg.append(a)
            r0 = 0
            for i, (src, n) in enumerate(specs):
                self.dma("act", self.A32[r0:r0 + n, a, 0:128], src[l].rearrange("(c p) -> c p", p=128), "cst2",
                         writes=[self.AB[a]], serialize=False, accum=(i > 0))
                r0 += n
            assert r0 == 76
            a2 = self.arena(1)
            stw.append(a2)
            self.dma("act", self.A32[0:CK, a2, 0:CW], self.conv_w[l], "cst2", writes=[self.AB[a2]], serialize=False)
        allst = [self.AB[i] for i in stg + stw]
        for b in allst:
            b.w = {"cst2": self.cnt["cst2"]}
        for l in range(DEPTH):
            pb = self.psum()
            self.op("pe", lambda l=l, pb=pb: nc.tensor.transpose(self.PS[:, pb, 0:76], self.A32[0:76, stg[l], 0:128], self.ident[0:76, 0:76]),
                    reads=allst + [C], writes=[self.PSB[pb]])
            self.op("dve", lambda l=l, pb=pb: nc.vector.tensor_copy(out=self.COLS[:, l, :], in_=self.PS[:, pb, 0:76]),
                    reads=[self.PSB[pb]], writes=[self.CONSTC])
            pb2 = self.psum()
            for c in range(4):
                self.op("pe", lambda l=l, pb2=pb2, c=c: nc.tensor.transpose(
                    self.PS[:, pb2, c * 32:c * 32 + CK], self.A32[0:CK, stw[l], c * 128:(c + 1) * 128], self.ident[0:CK, 0:CK]),
                    reads=allst + [C], writes=[self.PSB[pb2]], signal=(c == 3))
            self.op("dve", lambda l=l, pb2=pb2: nc.vector.tensor_copy(
                out=self.cw_col[:, l, :, :], in_=self.PS[:, pb2, 0:128].rearrange("p (c k) -> p c k", k=32)[:, :, 0:CK]),
                reads=[self.PSB[pb2]], writes=[self.CONSTC])
        for l in range(DEPTH):
            self.cdma(self.sgug_bc[:, l, :], self.sgu_ln_g[l:l + 1, :].partition_broadcast(128), q="act")
            self.cdma(self.sgub_bc[:, l, :], self.sgu_ln_b[l:l + 1, :].partition_broadcast(128), q="act")
            self.cdma(self.bv_bc[:, l, :], self.b_in[l:l + 1, 2048:2560].partition_broadcast(128), q="act")
            for g in range(4):
                self.cdma(self.wmix[:, l, g, :], self.pool_mix[l, g], q="pool", slow=False)
            for tb in range(4):
                self.cdma(self.bsrow[0:1, l, :, tb * 128:(tb + 1) * 128], self.sgu_b[l:l + 1, :, :], q="pool", slow=False)
        self.cdma(self.nfin_bc[:].rearrange("p a b -> p (a b)"), self.norm_final.rearrange("(o d) -> o d", o=1).partition_broadcast(128), q="act")
        self.cdma(self.R32[:], self.moe_router[0].rearrange("(kc p) e -> p kc e", p=128), q="act")
        for l in range(DEPTH):
            for h in range(4):
                a = self.arena(1)
                self.dma("act", self.A32[:, a, 0:128], self.sgu_w[l, h], "cst2", writes=[self.AB[a]], serialize=True)
                pb = self.psum()
                self.op("pe", lambda a=a, pb=pb: nc.tensor.transpose(self.PS[:, pb, 0:128], self.A32[:, a, 0:128], self.ident[:]),
                        reads=[self.AB[a], self.CONST], writes=[self.PSB[pb]])
                a2 = self.arena(1)
                self.op("dve", lambda a2=a2, pb=pb: nc.vector.tensor_copy(out=self.A32[:, a2, 0:128], in_=self.PS[:, pb, 0:128]),
                        reads=[self.PSB[pb]], writes=[self.AB[a2]])
                self.op("pool", lambda a2=a2, l=l, h=h: nc.gpsimd.affine_select(
                    out=self.wsT[:, l, h, :], in_=self.A32[:, a2, 0:128], pattern=[[1, 128]], compare_op=ALU.is_ge,
                    fill=0.0, base=0, channel_multiplier=-1), reads=[self.AB[a2]], writes=[C])

        stage_i = 0
        for it in self.items:
            if it.pieces is not None:
                continue
            dst_full = self.stream[it.chunk, :, it.off:it.off + it.R * it.W].rearrange("p (r w) -> p r w", w=it.W)
            if it.pieces is None:
                l, c = int(it.name[4]), int(it.name[6])
                hs = stage_i % 2
                stage_i += 1
                for k in range(CK):
                    self.op("dve", lambda l=l, c=c, k=k, hs=hs: nc.vector.tensor_scalar(
                        out=self.HID[:, hs, k // 4, (k % 4) * 128:(k % 4 + 1) * 128], in0=self.ident[:],
                        scalar1=self.cw_col[:, l, c, k:k + 1], scalar2=None, op0=ALU.mult),
                        reads=[C, self.CONST], writes=self.HIDB[hs])
                src = self.HID[:, hs, :, :].rearrange("p a b -> p (a b)")[:, 0:CK * 128].rearrange("p (r w) -> p r w", w=128)
                self.dma("sp", dst_full, src, "h_" + it.group, reads=self.HIDB[hs], writes=[self.hbuf[it.group]], serialize=False)


    def load_x(self, ti):
        for tb in range(4):
            r0 = ti * T + tb * 128
            self.dma("act", self.X[:, tb, :, :], self.x_d[r0:r0 + 128, :].rearrange("p (a b) -> p a b", a=2),
                     "xin%d" % tb, writes=self.XB[tb])

    def tile_program(self, ti):
        for l in range(DEPTH):
            self.cur_group = "L%dmix" % l
            self.mixer(l, ti)
            if self.upto == "mix%d" % l:
                break
            if l % 2 == 0:
                self.cur_group = "L%dffn" % l
                self.norm_to_hT("nffn_col", l, router=False)
                self.ffn("ffn", D_FF // 128, self.ffn_w_up[0], self.ffn_w_down[0], D_FF, None)
            else:
                self.norm_to_hT("nffn_col", l, router=True)
                for e in range(self.nexp):
                    self.cur_group = "E%d" % e
                    self.ffn("e%d" % e, D_FFE // 128, self.moe_w_up[0, e], self.moe_w_down[0, e], D_FFE, e)
            if self.upto == "ffn%d" % l:
                break
        self.final_norm(ti, plain=(self.upto != "all"))
        if not self.planning and ti + 1 < self.nt:
            self.load_x(ti + 1)

    def small(self):
        i = self.sm_next
        self.sm_next = (i + 1) % 32
        return i

    def row_rstd(self, src_aps, src_bufs):
        nc = self.nc
        n = len(src_aps)
        s = self.small()
        for i, (ap, b) in enumerate(zip(src_aps, src_bufs)):
            self.op("dve", lambda ap=ap, i=i, s=s: nc.vector.bn_stats(out=self.SM[:, s, i * 6:(i + 1) * 6], in_=ap()),
                    reads=[b], writes=[self.SMB[s]])
        s2 = self.small()
        self.op("dve", lambda s=s, s2=s2: nc.vector.bn_aggr(out=self.SM[:, s2, 0:2], in_=self.SM[:, s, 0:6 * n]),
                reads=[self.SMB[s]], writes=[self.SMB[s2]])
        return s2

    def rsqrt_eps(self, ap_fn, bufs):
        nc = self.nc
        self.op("dve", lambda: nc.vector.tensor_scalar(out=ap_fn(), in0=ap_fn(), scalar1=EPS, scalar2=None, op0=ALU.add),
                reads=bufs, writes=bufs)
        self.op("act", lambda: nc.scalar.activation(out=ap_fn(), in_=ap_fn(), func=AF.Sqrt), reads=bufs, writes=bufs)
        self.op("dve", lambda: nc.vector.reciprocal(out=ap_fn(), in_=ap_fn()), reads=bufs, writes=bufs)

    def rstd4(self, aps_fn, bufs_fn, use_ms):
        nc = self.nc
        sN = self.small()
        nb = self.SMB[sN]
        for tb in range(4):
            aps = aps_fn(tb)
            bufs = bufs_fn(tb)
            s = self.small()
            for i, ap in enumerate(aps):
                self.op("dve", lambda ap=ap, i=i, s=s: nc.vector.bn_stats(out=self.SM[:, s, i * 6:(i + 1) * 6], in_=ap()),
                        reads=bufs, writes=[self.SMB[s]])
            n = len(aps)
            self.op("dve", lambda s=s, tb=tb, n=n: nc.vector.bn_aggr(out=self.SM[:, sN, tb * 2:tb * 2 + 2], in_=self.SM[:, s, 0:6 * n]),
                    reads=[self.SMB[s]], writes=[nb])
        mv = lambda: self.SM[:, sN, 0:8].rearrange("p (b t) -> p b t", t=2)
        r = lambda: self.SM[:, sN, 8:12]
        if use_ms:
            self.op("dve", lambda: nc.vector.tensor_tensor(out=r(), in0=mv()[:, :, 0], in1=mv()[:, :, 0], op=ALU.mult), reads=[nb], writes=[nb])
            self.op("dve", lambda: nc.vector.tensor_tensor(out=r(), in0=r(), in1=mv()[:, :, 1], op=ALU.add), reads=[nb], writes=[nb])
        else:
            self.op("dve", lambda: nc.vector.tensor_copy(out=r(), in_=mv()[:, :, 1]), reads=[nb], writes=[nb])
        self.rsqrt_eps(r, [nb])
        return sN

    def norm_to_hT(self, gcol, l, router):
        nc = self.nc
        if self.planning:
            return
        gcol = getattr(self, gcol)
        sN = self.rstd4(lambda tb: [(lambda tb=tb, dh=dh: self.X[:, tb, dh, :]) for dh in range(2)], lambda tb: self.XB[tb], True)
        nb = self.SMB[sN]
        hn = []
        if not router:
            for tb in range(4):
                a = self.arena(1)
                hn.append(a)
                self.op("act", lambda a=a, tb=tb: nc.scalar.activation(
                    out=self.A32[:, a, :].bitcast(BF16)[:, 0:1024].rearrange("p (a b) -> p a b", a=2), in_=self.X[:, tb, :, :],
                    func=AF.Copy, scale=self.SM[:, sN, 8 + tb:9 + tb]),
                    reads=self.XB[tb] + [nb], writes=[self.AB[a]])
            for tb in range(4):
                a = hn[tb]
                pb = self.psum()
                for kc in range(8):
                    self.op("pe", lambda a=a, kc=kc, pb=pb: nc.tensor.transpose(
                        self.PS[:, pb, :].bitcast(BF16)[:, kc * 128:(kc + 1) * 128],
                        self.A32[:, a, :].bitcast(BF16)[:, kc * 128:(kc + 1) * 128], self.identb[:]),
                        reads=[self.AB[a], self.CONST], writes=[self.PSB[pb]], signal=(kc == 7))
                self.op("dve", lambda pb=pb, tb=tb: nc.vector.tensor_tensor(
                    out=self.HT[:, :, tb * 128:(tb + 1) * 128],
                    in0=self.PS[:, pb, :].bitcast(BF16)[:, 0:1024].rearrange("p (a b) -> p a b", a=8),
                    in1=gcol[:, l, :].unsqueeze(2).broadcast_to([128, 8, 128]), op=ALU.mult),
                    reads=[self.PSB[pb], self.CONST], writes=[self.HTB[tb]])
            return
        for tb in range(4):
            a = self.arena(2)
            hn.append(a)
            self.op("act", lambda a=a, tb=tb: nc.scalar.activation(
                out=self.A32[:, a:a + 2, :], in_=self.X[:, tb, :, :], func=AF.Copy, scale=self.SM[:, sN, 8 + tb:9 + tb]),
                reads=self.XB[tb] + [nb], writes=[self.AB[a], self.AB[a + 1]])
        for tb in range(4):
            a = hn[tb]
            banks = [self.psum(), self.psum()]
            for kc in range(8):
                pb = banks[kc // 4]
                j = kc % 4
                self.op("pe", lambda a=a, kc=kc, pb=pb, j=j: nc.tensor.transpose(
                    self.PS[:, pb, j * 128:(j + 1) * 128], self.A32[:, a + kc // 4, j * 128:(j + 1) * 128], self.ident[:]),
                    reads=[self.AB[a + kc // 4], self.CONST], writes=[self.PSB[pb]], signal=(j == 3))
            if router:
                h32 = self.arena(2)
            for kc in range(8):
                pb = banks[kc // 4]
                j = kc % 4
                if router:
                    dst = (lambda h32=h32, kc=kc, j=j: self.A32[:, h32 + kc // 4, j * 128:(j + 1) * 128])
                    dbuf = [self.AB[h32 + kc // 4]]
                else:
                    dst = (lambda kc=kc, tb=tb: self.HT[:, kc, tb * 128:(tb + 1) * 128])
                    dbuf = [self.HTB[tb]]
                if kc % 2 == 0:
                    self.op("dve", lambda dst=dst, pb=pb, j=j, kc=kc: nc.vector.tensor_scalar(
                        out=dst(), in0=self.PS[:, pb, j * 128:(j + 1) * 128], scalar1=gcol[:, l, kc:kc + 1], scalar2=None,
                        op0=ALU.mult), reads=[self.PSB[pb], self.CONST], writes=dbuf)
                else:
                    self.op("act", lambda dst=dst, pb=pb, j=j, kc=kc: nc.scalar.activation(
                        out=dst(), in_=self.PS[:, pb, j * 128:(j + 1) * 128], func=AF.Copy, scale=gcol[:, l, kc:kc + 1]),
                        reads=[self.PSB[pb], self.CONST], writes=dbuf)
            if router:
                for half in range(2):
                    self.op("act" if half else "dve", (lambda h32=h32, half=half, tb=tb: (
                        nc.scalar.copy(out=self.HT[:, half * 4:(half + 1) * 4, tb * 128:(tb + 1) * 128],
                                       in_=self.A32[:, h32 + half, :].rearrange("p (a b) -> p a b", a=4)) if half else
                        nc.vector.tensor_copy(out=self.HT[:, half * 4:(half + 1) * 4, tb * 128:(tb + 1) * 128],
                                              in_=self.A32[:, h32 + half, :].rearrange("p (a b) -> p a b", a=4)))),
                        reads=[self.AB[h32 + half]], writes=[self.HTB[tb]])
                self.router(tb, h32)

    def router(self, tb, h32):
        nc = self.nc
        pb = self.psum()
        for kc in range(8):
            self.op("pe", lambda kc=kc, pb=pb, h32=h32: nc.tensor.matmul(
                self.PS[:, pb, 0:NE], self.A32[:, h32 + kc // 4, (kc % 4) * 128:(kc % 4 + 1) * 128], self.R32[:, kc, :],
                start=(kc == 0), stop=(kc == 7)), reads=[self.AB[h32 + kc // 4], self.CONST], writes=[self.PSB[pb]],
                signal=(kc == 7))
        s = self.small()
        sb_ = self.SMB[s]
        SMs = self.SM

        def dv(fn, extra=()):
            self.op("dve", fn, reads=[sb_] + list(extra), writes=[sb_])
        s_lg, s_e1, s_l2, s_e2 = s, self.small(), self.small(), self.small()
        sc = self.small()
        bufs = [self.SMB[i] for i in (s_lg, s_e1, s_l2, s_e2, sc)]

        def dv2(fn, extra=()):
            self.op("dve", fn, reads=bufs + list(extra), writes=bufs)
        dv2(lambda: nc.vector.tensor_copy(out=SMs[:, s_lg, 0:NE], in_=self.PS[:, pb, 0:NE]), [self.PSB[pb]])
        dv2(lambda: nc.vector.reduce_max(out=SMs[:, sc, 0:1], in_=SMs[:, s_lg, 0:NE], axis=AX.X))
        dv2(lambda: nc.vector.tensor_scalar(out=SMs[:, s_e1, 0:NE], in0=SMs[:, s_lg, 0:NE], scalar1=SMs[:, sc, 0:1],
                                            scalar2=None, op0=ALU.is_equal))
        dv2(lambda: nc.vector.scalar_tensor_tensor(out=SMs[:, s_l2, 0:NE], in0=SMs[:, s_e1, 0:NE], scalar=-1e30,
                                                   in1=SMs[:, s_lg, 0:NE], op0=ALU.mult, op1=ALU.add))
        dv2(lambda: nc.vector.reduce_max(out=SMs[:, sc, 1:2], in_=SMs[:, s_l2, 0:NE], axis=AX.X))
        dv2(lambda: nc.vector.tensor_scalar(out=SMs[:, s_e2, 0:NE], in0=SMs[:, s_l2, 0:NE], scalar1=SMs[:, sc, 1:2],
                                            scalar2=None, op0=ALU.is_equal))
        dv2(lambda: nc.vector.tensor_tensor(out=SMs[:, sc, 2:3], in0=SMs[:, sc, 1:2], in1=SMs[:, sc, 0:1], op=ALU.subtract))
        self.op("act", lambda: nc.scalar.activation(out=SMs[:, sc, 3:4], in_=SMs[:, sc, 2:3], func=AF.Exp),
                reads=bufs, writes=bufs)
        dv2(lambda: nc.vector.tensor_scalar(out=SMs[:, sc, 4:5], in0=SMs[:, sc, 3:4], scalar1=1.0, scalar2=None, op0=ALU.add))
        dv2(lambda: nc.vector.reciprocal(out=SMs[:, sc, 5:6], in_=SMs[:, sc, 4:5]))
        dv2(lambda: nc.vector.tensor_tensor(out=SMs[:, sc, 6:7], in0=SMs[:, sc, 3:4], in1=SMs[:, sc, 5:6], op=ALU.mult))
        dv2(lambda: nc.vector.tensor_scalar(out=SMs[:, s_e1, 0:NE], in0=SMs[:, s_e1, 0:NE], scalar1=SMs[:, sc, 5:6],
                                            scalar2=None, op0=ALU.mult))
        self.op("dve", lambda: nc.vector.scalar_tensor_tensor(out=self.GATE[:, tb, :], in0=SMs[:, s_e2, 0:NE],
                                                              scalar=SMs[:, sc, 6:7], in1=SMs[:, s_e1, 0:NE],
                                                              op0=ALU.mult, op1=ALU.add),
                reads=bufs, writes=[self.GATEB[tb]])

    def hT_specs(self, it, r_of_kc, c0, n=128):
        specs = []
        for kc in range(8):
            specs.append(((lambda kc=kc: self.iap(it, r_of_kc(kc), c0, n)),
                          (lambda kc=kc: self.HT[:, kc, :]),
                          [self.ibuf(it)] + self.HTB if not self.planning else []))
        return specs

    def gelu(self, out_fn, in_fn, bias_fn, reads, writes):
        nc = self.nc
        if GELU_FUNC == "native":
            if bias_fn is None:
                self.op("act", lambda: nc.scalar.activation(out=out_fn(), in_=in_fn(), func=AF.Gelu_apprx_tanh),
                        reads=reads, writes=writes)
            else:
                self.op("act", lambda: nc.scalar.activation(out=out_fn(), in_=in_fn(), func=AF.Gelu_apprx_tanh,
                                                            bias=bias_fn(), scale=1.0), reads=reads, writes=writes)
            return
        a = self.arena(2)
        xa = lambda: self.A32[:, a, :]
        ta = lambda: self.A32[:, a + 1, :]
        ab = [self.AB[a], self.AB[a + 1]]
        if bias_fn is None:
            self.op("act", lambda: nc.scalar.copy(out=xa(), in_=in_fn()), reads=reads, writes=ab)
        else:
            self.op("act", lambda: nc.scalar.activation(out=xa(), in_=in_fn(), func=AF.Identity, bias=bias_fn(), scale=1.0),
                    reads=reads, writes=ab)
        self.op("dve", lambda: nc.vector.tensor_tensor(out=ta(), in0=xa(), in1=xa(), op=ALU.mult), reads=ab, writes=ab)
        self.op("dve", lambda: nc.vector.tensor_scalar(out=ta(), in0=ta(), scalar1=0.044715, scalar2=1.0, op0=ALU.mult,
                                                       op1=ALU.add), reads=ab, writes=ab)
        self.op("dve", lambda: nc.vector.tensor_tensor(out=ta(), in0=ta(), in1=xa(), op=ALU.mult), reads=ab, writes=ab)
        self.op("act", lambda: nc.scalar.activation(out=ta(), in_=ta(), func=AF.Sigmoid, scale=1.5957691216057308),
                reads=ab, writes=ab)
        self.op("dve", lambda: nc.vector.tensor_tensor(out=out_fn(), in0=ta(), in1=xa(), op=ALU.mult), reads=ab, writes=writes)

    def mixer(self, l, ti):
        nc = self.nc
        P = self.planning

        def colsrc(w, c0, n):
            return w[:, c0:c0 + n].rearrange("(kc p) j -> p kc j", p=128)
        self.norm_to_hT("nmix_col", l, router=False)
        itA = self.item("win_conv%d" % l, 8, 1024, [(0, 8, 0, 1024, colsrc(self.w_in[l], 0, 1024))])
        for c in range(4):
            ba, bg = self.psum(), self.psum()
            if not P:
                self.mm_group(lambda ba=ba: self.PS[:, ba, :], self.PSB[ba], self.hT_specs(itA, lambda kc: kc, c * 128))
                self.mm_group(lambda bg=bg: self.PS[:, bg, :], self.PSB[bg], self.hT_specs(itA, lambda kc: kc, 512 + c * 128))
                a = self.arena(1)
                self.op("act", lambda a=a, bg=bg, c=c: nc.scalar.activation(
                    out=self.A32[:, a, :], in_=self.PS[:, bg, :], func=AF.Sigmoid, bias=self.bin_col[:, l, 4 + c:5 + c], scale=1.0),
                    reads=[self.PSB[bg], self.CONST], writes=[self.AB[a]])
                self.op("dve", lambda a=a, ba=ba, c=c: nc.vector.scalar_tensor_tensor(
                    out=self.HC[l][:, c, HALO_C:HALO_C + T], in0=self.PS[:, ba, :], scalar=self.bin_col[:, l, c:c + 1],
                    in1=self.A32[:, a, :], op0=ALU.add, op1=ALU.mult),
                    reads=[self.PSB[ba], self.AB[a], self.CONST], writes=[self.HCB[l][c]])
        rc, rq = [], []
        for c in range(4):
            itD = self.item("diag%d_%d" % (l, c), CK, 128, None)
            if P:
                continue
            bc = self.psum()
            specs = []
            for k in range(CK):
                specs.append(((lambda k=k, itD=itD: self.iap(itD, k, 0, 128)),
                              (lambda k=k, c=c: self.HC[l][:, c, k:k + T]),
                              [self.ibuf(itD), self.HCB[l][c], self.HCH[l][c]]))
            self.mm_group(lambda bc=bc: self.PS[:, bc, :], self.PSB[bc], specs)
            a = self.arena(2)
            rc.append(a)
            rq.append(a + 1)
            self.op("act", lambda a=a, bc=bc, c=c: nc.scalar.activation(
                out=self.A32[:, a, :], in_=self.PS[:, bc, :], func=AF.Identity, bias=self.convb_col[:, l, c:c + 1], scale=1.0),
                reads=[self.PSB[bc], self.CONST], writes=[self.AB[a]])
            self.op("act", lambda a=a, bc=bc, c=c: nc.scalar.activation(
                out=self.A32[:, a + 1, :], in_=self.PS[:, bc, :], func=AF.Square, bias=self.convb_col[:, l, c:c + 1], scale=1.0),
                reads=[self.PSB[bc], self.CONST], writes=[self.AB[a + 1]])
            self.op("act", lambda c=c: nc.scalar.copy(out=self.HC[l][:, c, 0:HALO_C], in_=self.HC[l][:, c, T:T + HALO_C]),
                    reads=[self.HCB[l][c]], writes=[self.HCH[l][c]])
        if not P:
            bm, bq = self.psum(), self.psum()
            self.mm_group(lambda: self.PS[:, bm, :], self.PSB[bm],
                          [((lambda: self.onesm[:]), (lambda c=c: self.A32[:, rc[c], :]), [self.AB[rc[c]], self.CONST]) for c in range(4)])
            self.mm_group(lambda: self.PS[:, bq, :], self.PSB[bq],
                          [((lambda: self.onesm[:]), (lambda c=c: self.A32[:, rq[c], :]), [self.AB[rq[c]], self.CONST]) for c in range(4)])
            am = self.arena(2)
            mean = lambda: self.A32[:, am, :]
            rstd = lambda: self.A32[:, am + 1, :]
            mb = [self.AB[am], self.AB[am + 1]]
            self.op("dve", lambda: nc.vector.tensor_copy(out=mean(), in_=self.PS[:, bm, :]), reads=[self.PSB[bm]], writes=[mb[0]])
            self.op("dve", lambda: nc.vector.tensor_tensor(out=rstd(), in0=mean(), in1=mean(), op=ALU.mult), reads=[mb[0]], writes=[mb[1]])
            self.op("dve", lambda: nc.vector.tensor_tensor(out=rstd(), in0=self.PS[:, bq, :], in1=rstd(), op=ALU.subtract),
                    reads=[self.PSB[bq], mb[1]], writes=[mb[1]])
            self.rsqrt_eps(rstd, [mb[1]])
            for c in range(4):
                self.op("dve", lambda c=c: nc.vector.tensor_tensor(out=self.A32[:, rc[c], :], in0=self.A32[:, rc[c], :], in1=mean(), op=ALU.subtract),
                        reads=[self.AB[rc[c]], mb[0]], writes=[self.AB[rc[c]]])
                self.op("dve", lambda c=c: nc.vector.tensor_tensor(out=self.A32[:, rc[c], :], in0=self.A32[:, rc[c], :], in1=rstd(), op=ALU.mult),
                        reads=[self.AB[rc[c]], mb[1]], writes=[self.AB[rc[c]]])
                self.op("act", lambda c=c: nc.scalar.activation(out=self.CT[:, c, :], in_=self.A32[:, rc[c], :], func=AF.Silu,
                                                                bias=self.clnb_col[:, l, c:c + 1], scale=self.clng_col[:, l, c:c + 1]),
                        reads=[self.AB[rc[c]], self.CONST], writes=[self.CTB[c]])
        itP = self.item("win_pool%d" % l, 8, 512, [(0, 8, 0, 512, colsrc(self.w_in[l], 1024, 512))])
        PPl = self.PP[l] if not P else None
        L = HALO_P + T
        if not P:
            for g, w in enumerate(POOL_W):
                pb = self.psum()
                self.mm_group(lambda pb=pb: self.PS[:, pb, :], self.PSB[pb], self.hT_specs(itP, lambda kc: kc, g * 128))
                self.op("act", lambda pb=pb, g=g: nc.scalar.activation(
                    out=PPl[:, g, HALO_P:HALO_P + T], in_=self.PS[:, pb, :], func=AF.Identity, bias=self.bin_col[:, l, 8 + g:9 + g], scale=1.0),
                    reads=[self.PSB[pb], self.CONST], writes=[self.PPB[l][g]])
        itU = self.item("win_u%d" % l, 8, 512, [(0, 8, 0, 512, colsrc(self.w_in[l], 1536, 512))])
        if not P:
            for h in range(4):
                pb = self.psum()
                self.mm_group(lambda pb=pb: self.PS[:, pb, :], self.PSB[pb], self.hT_specs(itU, lambda kc: kc, h * 128))
                self.gelu(lambda h=h: self.UT[:, h, :], lambda pb=pb: self.PS[:, pb, :], lambda h=h: self.bin_col[:, l, 12 + h:13 + h],
                          [self.PSB[pb], self.CONST], [self.UTB[h]])
            for g, w in enumerate(POOL_W):
                src_fn = (lambda g=g: PPl[:, g, :])
                src_b = [self.PPB[l][g], self.PPH[l][g]]
                tmp = [(self.PS1, self.PS1B), (self.PS2, self.PS2B)]
                sh = 1
                lvl = 0
                lo = 0
                while sh < w:
                    dstT, dstB = tmp[lvl % 2]
                    lo2 = lo + sh
                    self.op("dve", lambda src_fn=src_fn, dstT=dstT, lo2=lo2, sh=sh: nc.vector.tensor_tensor(
                        out=dstT[:, lo2:L], in0=src_fn()[:, lo2:L], in1=src_fn()[:, lo2 - sh:L - sh], op=ALU.add),
                        reads=src_b, writes=[dstB])
                    src_fn = (lambda dstT=dstT: dstT[:, 0:L])
                    src_b = [dstB]
                    lo = lo2
                    sh *= 2
                    lvl += 1
                assert lo == w - 1
                self.op("dve", lambda src_fn=src_fn, g=g, w=w: nc.vector.scalar_tensor_tensor(
                    out=self.DT[:, g, :], in0=src_fn()[:, HALO_P:L], scalar=1.0 / w, in1=PPl[:, g, HALO_P:L],
                    op0=ALU.mult, op1=ALU.subtract), reads=src_b + [self.PPB[l][g]], writes=[self.DTB[g]])
                if ti == 0:
                    a = self.arena(1)
                    self.op("dve", lambda src_fn=src_fn, g=g, a=a: nc.vector.tensor_tensor(
                        out=self.A32[:, a, 0:16], in0=src_fn()[:, HALO_P:HALO_P + 16], in1=self.icnt[:, g, :], op=ALU.mult),
                        reads=src_b + [self.CONST], writes=[self.AB[a]])
                    self.op("dve", lambda g=g, a=a: nc.vector.tensor_tensor(
                        out=self.DT[:, g, 0:16], in0=self.A32[:, a, 0:16], in1=PPl[:, g, HALO_P:HALO_P + 16], op=ALU.subtract),
                        reads=[self.AB[a], self.PPB[l][g]], writes=[self.DTB[g]])
                self.op("act", lambda g=g: nc.scalar.copy(out=PPl[:, g, 0:HALO_P], in_=PPl[:, g, T:T + HALO_P]),
                        reads=[self.PPB[l][g]], writes=[self.PPH[l][g]])
        itV = self.item("win_v%d" % l, 8, 512, [(0, 8, 0, 512, colsrc(self.w_in[l], 2048, 512))])
        if not P:
            vas = []
            for tb in range(4):
                pb = self.psum()
                specs = [((lambda kc=kc, tb=tb: self.HT[:, kc, tb * 128:(tb + 1) * 128]),
                          (lambda kc=kc: self.iap(itV, kc, 0, 512)), [self.ibuf(itV)] + self.HTB) for kc in range(8)]
                self.mm_group(lambda pb=pb: self.PS[:, pb, :], self.PSB[pb], specs)
                a = self.arena(1)
                vas.append(a)
                va = (lambda a=a: self.A32[:, a, :])
                vb = [self.AB[a]]
                self.op("dve", lambda pb=pb, va=va: nc.vector.tensor_tensor(out=va(), in0=self.PS[:, pb, :], in1=self.bv_bc[:, l, :], op=ALU.add),
                        reads=[self.PSB[pb], self.CONST], writes=vb)
                self.gelu(va, va, None, vb, vb)
            for g in range(4):
                pb2 = self.psum()
                self.mm_group(lambda pb2=pb2: self.PS[:, pb2, :], self.PSB[pb2],
                              [((lambda g=g: self.wmix[:, l, g, :]), (lambda g=g: self.DT[:, g, :]), [self.CONST, self.DTB[g]])])
                self.op("act", lambda pb2=pb2, g=g: nc.scalar.activation(
                    out=self.YP[:, g, :], in_=self.PS[:, pb2, :], func=AF.Copy, scale=self.pscale_col[:, l, g:g + 1]),
                    reads=[self.PSB[pb2], self.CONST], writes=[self.YPB[g]])
            sN = self.rstd4(lambda tb: [(lambda tb=tb: self.A32[:, vas[tb], :])], lambda tb: [self.AB[vas[tb]]], False)
            nb = self.SMB[sN]
            for tb in range(4):
                a = vas[tb]
                va = (lambda a=a: self.A32[:, a, :])
                vb = [self.AB[a]]
                self.op("dve", lambda va=va, tb=tb: nc.vector.tensor_scalar(
                    out=va(), in0=va(), scalar1=self.SM[:, sN, 2 * tb:2 * tb + 1], scalar2=self.SM[:, sN, 8 + tb:9 + tb],
                    op0=ALU.subtract, op1=ALU.mult), reads=vb + [nb], writes=vb)
                self.op("dve", lambda va=va: nc.vector.tensor_tensor(out=va(), in0=va(), in1=self.sgug_bc[:, l, :], op=ALU.mult),
                        reads=vb + [self.CONST], writes=vb)
                self.op("dve", lambda va=va, tb=tb: nc.vector.tensor_tensor(out=self.VL[:, tb, :], in0=va(), in1=self.sgub_bc[:, l, :], op=ALU.add),
                        reads=vb + [self.CONST], writes=[self.VLB[tb]])
            for h in range(4):
                pb = self.psum()
                self.op("pe", lambda pb=pb, h=h: nc.tensor.matmul(
                    self.PS[:, pb, :], self.onesrow[0:1, :], self.bsrow[0:1, l, h, :], start=True, stop=False),
                    reads=[self.CONST], writes=[self.PSB[pb]], signal=False)
                for tb in range(4):
                    self.op("pe", lambda pb=pb, tb=tb, h=h: nc.tensor.matmul(
                        self.PS[:, pb, tb * 128:(tb + 1) * 128], self.VL[:, tb, h * 128:(h + 1) * 128], self.wsT[:, l, h, :],
                        start=False, stop=(tb == 3)), reads=[self.VLB[tb], self.CONST], writes=[self.PSB[pb]], signal=(tb == 3))
                self.op("dve", lambda pb=pb, h=h: nc.vector.tensor_tensor(out=self.YS[:, h, :], in0=self.PS[:, pb, :], in1=self.UT[:, h, :], op=ALU.mult),
                        reads=[self.PSB[pb], self.UTB[h]], writes=[self.YSB[h]])
        outs_w = (self.conv_out[l], self.pool_out[l], self.sgu_out[l])
        for dc in range(8):
            itG = self.item("gate%d_%d" % (l, dc), 24, 128,
                            [(i * 8, 8, 0, 128, colsrc(self.w_gate[l], i * 1024 + dc * 128, 128)) for i in range(3)])
            if P:
                self.item("outs%d_%d" % (l, dc), 12, 128,
                          [(i * 4, 4, 0, 128, colsrc(outs_w[i], dc * 128, 128)) for i in range(3)])
                continue
            ga = []
            for i in range(3):
                pb = self.psum()
                self.mm_group(lambda pb=pb: self.PS[:, pb, :], self.PSB[pb], self.hT_specs(itG, lambda kc, i=i: i * 8 + kc, 0))
                a = self.arena(1)
                ga.append(a)
                self.op("act", lambda pb=pb, a=a, i=i: nc.scalar.activation(
                    out=self.A32[:, a, :], in_=self.PS[:, pb, :], func=AF.Sigmoid, bias=self.bgate_col[:, l, i * 8 + dc:i * 8 + dc + 1], scale=1.0),
                    reads=[self.PSB[pb], self.CONST], writes=[self.AB[a]])
            itO = self.item("outs%d_%d" % (l, dc))
            srcs = ((self.CT, self.CTB), (self.YP, self.YPB), (self.YS, self.YSB))
            for i in range(3):
                pb = self.psum()
                sT, sB = srcs[i]
                specs = [((lambda c=c, i=i: self.iap(itO, i * 4 + c, 0, 128)), (lambda c=c, sT=sT: sT[:, c, :]), [self.ibuf(itO), sB[c]])
                         for c in range(4)]
                self.mm_group(lambda pb=pb: self.PS[:, pb, :], self.PSB[pb], specs)
                a = ga[i]
                self.op("dve", lambda pb=pb, a=a: nc.vector.tensor_tensor(out=self.A32[:, a, :], in0=self.A32[:, a, :], in1=self.PS[:, pb, :], op=ALU.mult),
                        reads=[self.PSB[pb], self.AB[a]], writes=[self.AB[a]])
            self.op("dve", lambda ga=ga: nc.vector.tensor_tensor(out=self.A32[:, ga[0], :], in0=self.A32[:, ga[0], :], in1=self.A32[:, ga[1], :], op=ALU.add),
                    reads=[self.AB[ga[0]], self.AB[ga[1]]], writes=[self.AB[ga[0]]])
            self.op("dve", lambda ga=ga, dc=dc: nc.vector.tensor_tensor(out=self.HID[:, 0, dc, :], in0=self.A32[:, ga[0], :], in1=self.A32[:, ga[2], :], op=ALU.add),
                    reads=[self.AB[ga[0]], self.AB[ga[2]]], writes=[self.HIDB[0][dc]])
        for dh in range(2):
            itW = self.item("wo%d_%d" % (l, dh), 8, 512, [(0, 8, 0, 512, colsrc(self.w_o[l], dh * 512, 512))])
            if P:
                continue
            for tb in range(4):
                pb = self.psum()
                specs = [((lambda kc=kc, tb=tb: self.HID[:, 0, kc, tb * 128:(tb + 1) * 128]),
                          (lambda kc=kc, itW=itW: self.iap(itW, kc, 0, 512)), [self.ibuf(itW), self.HIDB[0][kc]]) for kc in range(8)]
                self.mm_group(lambda pb=pb: self.PS[:, pb, :], self.PSB[pb], specs)
                self.op("dve", lambda pb=pb, tb=tb, dh=dh: nc.vector.tensor_tensor(
                    out=self.X[:, tb, dh, :], in0=self.X[:, tb, dh, :], in1=self.PS[:, pb, :], op=ALU.add),
                    reads=[self.PSB[pb], self.XB[tb][dh]], writes=[self.XB[tb][dh]])

    def ffn(self, pfx, nf, w_up, w_down, F, expert):
        nc = self.nc
        P = self.planning
        segs = []
        f = 0
        while f < nf:
            n = min(8, nf - f)
            segs.append((f, n))
            f += n
        ups = {}

        def up_item(q):
            nq = min(4, nf - q * 4)
            pieces = [(0, 8, 0, nq * 128, w_up[:, q * 512:q * 512 + nq * 128].rearrange("(kc p) j -> p kc j", p=128)),
                      (0, 8, nq * 128, nq * 128, w_up[:, F + q * 512:F + q * 512 + nq * 128].rearrange("(kc p) j -> p kc j", p=128))]
            return self.item("%s_up%d" % (pfx, q), 8, 2 * nq * 128, pieces)

        def do_up(si):
            f0, n = segs[si]
            hb = si % 2
            for jj in range(n):
                fch = f0 + jj
                q = fch // 4
                if (fch % 4) == 0:
                    ups[q] = up_item(q)
                if P:
                    continue
                it = ups[q]
                j4 = fch % 4
                ba, bb = self.psum(), self.psum()
                self.mm_group(lambda ba=ba: self.PS[:, ba, :], self.PSB[ba], self.hT_specs(it, lambda kc: kc, j4 * 128))
                self.mm_group(lambda bb=bb: self.PS[:, bb, :], self.PSB[bb], self.hT_specs(it, lambda kc: kc, it.W // 2 + j4 * 128))
                a = self.arena(1)
                self.op("act", lambda a=a, ba=ba: nc.scalar.activation(out=self.A32[:, a, :], in_=self.PS[:, ba, :], func=AF.Silu),
                        reads=[self.PSB[ba]], writes=[self.AB[a]])
                self.op("dve", lambda a=a, bb=bb, hb=hb, jj=jj: nc.vector.tensor_tensor(
                    out=self.HID[:, hb, jj, :], in0=self.A32[:, a, :], in1=self.PS[:, bb, :], op=ALU.mult),
                    reads=[self.AB[a], self.PSB[bb]], writes=[self.HIDB[hb][jj]])

        def do_down(si):
            f0, n = segs[si]
            hb = si % 2
            it = self.item("%s_down%d" % (pfx, si), n, 1024,
                           [(0, n, 0, 1024, w_down[f0 * 128:(f0 + n) * 128, :].rearrange("(r p) j -> p r j", p=128))])
            if P:
                return
            for tb in range(4):
                for dh in range(2):
                    pb = self.psum()
                    specs = [((lambda jj=jj, tb=tb: self.HID[:, hb, jj, tb * 128:(tb + 1) * 128]),
                              (lambda jj=jj, dh=dh: self.iap(it, jj, dh * 512, 512)), [self.ibuf(it), self.HIDB[hb][jj]]) for jj in range(n)]
                    self.mm_group(lambda pb=pb: self.PS[:, pb, :], self.PSB[pb], specs)
                    if expert is None:
                        self.op("dve", lambda pb=pb, tb=tb, dh=dh: nc.vector.tensor_tensor(
                            out=self.X[:, tb, dh, :], in0=self.X[:, tb, dh, :], in1=self.PS[:, pb, :], op=ALU.add),
                            reads=[self.PSB[pb], self.XB[tb][dh]], writes=[self.XB[tb][dh]])
                    else:
                        self.op("dve", lambda pb=pb, tb=tb, dh=dh: nc.vector.scalar_tensor_tensor(
                            out=self.X[:, tb, dh, :], in0=self.PS[:, pb, :], scalar=self.GATE[:, tb, expert:expert + 1],
                            in1=self.X[:, tb, dh, :], op0=ALU.mult, op1=ALU.add),
                            reads=[self.PSB[pb], self.XB[tb][dh], self.GATEB[tb]], writes=[self.XB[tb][dh]])
        do_up(0)
        for si in range(len(segs)):
            if si + 1 < len(segs):
                do_up(si + 1)
            do_down(si)

    def final_norm(self, ti, plain=False):
        nc = self.nc
        if self.planning:
            return
        if not plain:
            sN = self.rstd4(lambda tb: [(lambda tb=tb, dh=dh: self.X[:, tb, dh, :]) for dh in range(2)], lambda tb: self.XB[tb], True)
            nb = self.SMB[sN]
        for tb in range(4):
            a = self.arena(2)
            ab = [self.AB[a], self.AB[a + 1]]
            if plain:
                self.op("dve", lambda a=a, tb=tb: nc.vector.tensor_copy(out=self.A32[:, a:a + 2, :], in_=self.X[:, tb, :, :]),
                        reads=self.XB[tb], writes=ab)
            else:
                self.op("dve", lambda a=a, tb=tb: nc.vector.scalar_tensor_tensor(
                    out=self.A32[:, a:a + 2, :], in0=self.X[:, tb, :, :], scalar=self.SM[:, sN, 8 + tb:9 + tb], in1=self.nfin_bc[:],
                    op0=ALU.mult, op1=ALU.mult), reads=self.XB[tb] + [nb, self.CONST], writes=ab)
            r0 = ti * T + tb * 128
            self.dma("act", self.out_d[r0:r0 + 128, :].rearrange("p (a b) -> p a b", a=2), self.A32[:, a:a + 2, :],
                     "out%d" % tb, reads=ab)


_CACHE = {}
WNAMES = ["norm_mix", "w_in", "b_in", "conv_w", "conv_b", "conv_ln_g", "conv_ln_b", "conv_out", "pool_mix", "pool_scale",
          "pool_out", "sgu_ln_g", "sgu_ln_b", "sgu_w", "sgu_b", "sgu_out", "w_gate", "b_gate", "w_o", "norm_ffn",
          "ffn_w_up", "ffn_w_down", "moe_router", "moe_w_up", "moe_w_down", "norm_final"]


def run(inputs, nt, ncores, nexp=NE, upto="all", trace=False):
    key = (nt, nexp, upto)
    if key not in _CACHE:
        _CACHE[key] = Builder(nt, nt * T, nexp, upto).build()
    nc = _CACHE[key]
    x = np.asarray(inputs["x"], dtype=np.float32)
    shared = {k: np.ascontiguousarray(np.asarray(inputs[k], dtype=np.float32)) for k in WNAMES}
    in_maps = []
    for b in range(ncores):
        m = dict(shared)
        m["x"] = np.ascontiguousarray(x[b, :nt * T])
        in_maps.append(m)
    res = run_bass_kernel_spmd(nc, in_maps, core_ids=list(range(ncores)), trace=trace)
    out = np.stack([np.asarray(r["out"]) for r in res.results], axis=0)
    return out, res


def kernel(**inputs):
    out, _ = run(inputs, nt=8, ncores=8)
    return out.astype(np.float32)
```

```python
import contextlib
import numpy as np
import concourse.bass as bass
import concourse.mybir as mybir
from concourse.bass_utils import run_bass_kernel_spmd

F32 = mybir.dt.float32
BF16 = mybir.dt.bfloat16
AF = mybir.ActivationFunctionType
ALU = mybir.AluOpType
AX = mybir.AxisListType

D = 1024
T = 512
DEPTH = 2
CW = 512
CK = 31
HALO_C = CK - 1
POOL_W = (2, 4, 8, 16)
HALO_P = 15
D_FF = 2816
NE = 8
D_FFE = 3584
EPS = 1e-6
CHUNK = 8192
NSLOT = 4
NARENA = 12
GELU_FUNC = "native"


class Buf:
    __slots__ = ("w", "r", "name")

    def __init__(self, name=""):
        self.w = {}
        self.r = {}
        self.name = name


class Item:
    __slots__ = ("name", "R", "W", "pieces", "chunk", "off", "group")

    def __init__(self, name, R, W, pieces, group):
        self.name, self.R, self.W, self.pieces, self.group = name, R, W, pieces, group
        self.chunk = None
        self.off = None


class Builder:
    def __init__(self, nt, seq_total, nexp=NE, upto="all"):
        self.nt = nt
        self.S = nt * T
        self.nexp = nexp
        self.upto = upto
        self.nc = bass.Bass("TRN2", target_bir_lowering=False)
        self.planning = False
        self.items = []
        self.item_by_name = {}
        self.item_pos = 0
        self.cur_group = None
        self.ps_next = 0
        self.ar_next = 0
        self.sm_next = 0

    def setup_tracker(self, stack):
        nc = self.nc
        self.eng = {"pe": nc.tensor, "act": nc.scalar, "dve": nc.vector, "pool": nc.gpsimd, "sp": nc.sync}
        self.sems = {}
        self.cnt = {}
        self.seen = {e: {} for e in self.eng}
        self.stack = stack
        for e in self.eng:
            self.newsem(e)

    def newsem(self, key):
        self.sems[key] = self.stack.enter_context(self.nc.semaphore("s_" + key))
        self.cnt[key] = 0

    def _waits(self, e, reads, writes, extra=()):
        deps = {}

        def add(tok):
            if tok is None:
                return
            k, v = tok
            if deps.get(k, 0) < v:
                deps[k] = v
        for b in reads:
            for k, v in b.w.items():
                add((k, v))
        for b in writes:
            for k, v in b.w.items():
                add((k, v))
            for k, v in b.r.items():
                add((k, v))
        for tok in extra:
            add(tok)
        for k, v in deps.items():
            if k == e:
                if e == "pe" or e == "sp":
                    continue
            if self.seen[e].get(k, 0) >= v:
                continue
            self.eng[e].wait_ge(self.sems[k], v)
            self.seen[e][k] = v

    def op(self, e, fn, reads=(), writes=(), signal=True):
        if self.planning:
            return None
        if self.CONST in reads:
            reads = list(reads) + [self.CONSTP, self.CONSTQ, self.CONSTC]
        self._waits(e, reads, writes)
        ins = fn()
        tok = (e, self.cnt[e] + 1)
        if signal:
            ins.then_inc(self.sems[e], 1)
            self.cnt[e] += 1
        for b in writes:
            b.w = {tok[0]: tok[1]}
            b.r = {}
        for b in reads:
            if b.r.get(e, 0) < tok[1]:
                b.r[e] = tok[1]
        return ins

    def dma(self, q, out, in_, key, reads=(), writes=(), serialize=True, slow=False, accum=False, extra_toks=()):
        if self.planning:
            return
        extra = [(key, self.cnt[key])] if (serialize and self.cnt[key] > 0) else []
        extra = extra + list(extra_toks)
        self._waits(q, reads, writes if not accum else (), extra)
        self.cnt[key] += 16
        tok = (key, self.cnt[key])
        if slow:
            self.eng[q].dma_start(out=out, in_=in_, allow_slow_non_contiguous=True).then_inc(self.sems[key], 16)
        else:
            self.eng[q].dma_start(out=out, in_=in_).then_inc(self.sems[key], 16)
        for b in writes:
            if accum:
                b.w[key] = tok[1]
            else:
                b.w = {key: tok[1]}
                b.r = {}
        for b in reads:
            if b.r.get(key, 0) < tok[1]:
                b.r[key] = tok[1]

    def item(self, name, R=None, W=None, pieces=None):
        if self.planning:
            it = Item(name, R, W, pieces, self.cur_group)
            self.items.append(it)
            self.item_by_name[name] = it
            return it
        it = self.item_by_name[name]
        gchunk = self.tile_idx * self.nchunk + it.chunk
        assert gchunk >= self.ring_cur, (name, gchunk, self.ring_cur)
        if gchunk > self.ring_cur or self.ring_issued == 0:
            self.ring_cur = gchunk
            lim = min(gchunk - 1 + NSLOT, self.nt * self.nchunk - 1)
            while self.ring_issued <= lim:
                g = self.ring_issued
                ch = g % self.nchunk
                slot = g % NSLOT
                used = self.chunk_used[ch]
                sbuf_ = self.slotbuf[slot]
                if g < self.nchunk:
                    pre = list(sbuf_.w.items()) + list(sbuf_.r.items())
                    first = True
                    for cit in self.chunk_items[ch]:
                        sz = cit.R * cit.W
                        if cit.pieces is None:
                            self.dma("sp", self.RING[:, slot, cit.off:cit.off + sz], self.stream[ch, :, cit.off:cit.off + sz],
                                     "slot%d" % slot, reads=[self.hbuf[cit.group]], writes=[sbuf_], accum=not first, extra_toks=pre)
                            first = False
                        else:
                            view = self.RING[:, slot, cit.off:cit.off + sz].rearrange("p (r w) -> p r w", w=cit.W)
                            for (r0, nr, c0, ncw, src) in cit.pieces:
                                self.dma("pool", view[:, r0:r0 + nr, c0:c0 + ncw], src, "slotc%d" % slot, writes=[sbuf_],
                                         serialize=False, accum=not first, extra_toks=pre)
                                first = False
                    for cit in self.chunk_items[ch]:
                        if cit.pieces is not None:
                            sz = cit.R * cit.W
                            self.dma("sp", self.stream[ch, :, cit.off:cit.off + sz], self.RING[:, slot, cit.off:cit.off + sz],
                                     "wb%d" % slot, reads=[sbuf_], writes=[self.wbbuf[ch]], serialize=False, accum=True)
                else:
                    self.dma("sp", self.RING[:, slot, 0:used], self.stream[ch, :, 0:used], "slot%d" % slot,
                             reads=[self.wbbuf[ch]], writes=[sbuf_])
                self.ring_issued += 1
        return it

    def iap(self, it, r, c0, n):
        gchunk = self.tile_idx * self.nchunk + it.chunk
        assert gchunk >= self.ring_cur, ("stale ring item", it.name)
        slot = gchunk % NSLOT
        o = it.off + r * it.W + c0
        return self.RING[:, slot, o:o + n]

    def ibuf(self, it):
        gchunk = self.tile_idx * self.nchunk + it.chunk
        return self.slotbuf[gchunk % NSLOT]

    def pack_items(self):
        ch, off = 0, 0
        self.chunk_groups = {}
        self.chunk_items = {}
        self.chunk_used = {}
        for it in self.items:
            sz = it.R * it.W
            assert sz <= CHUNK
            if off + sz > CHUNK:
                ch += 1
                off = 0
            it.chunk, it.off = ch, off
            off += sz
            self.chunk_groups.setdefault(ch, set()).add(it.group)
            self.chunk_items.setdefault(ch, []).append(it)
            self.chunk_used[ch] = off
        self.nchunk = ch + 1

    def psum(self):
        i = self.ps_next
        self.ps_next = (i + 1) % 8
        return i

    def arena(self, n=1):
        i = self.ar_next
        if i + n > NARENA:
            i = 0
        self.ar_next = (i + n) % NARENA
        return i

    def mm_group(self, bank_ap, bankbuf, specs, extra_reads=()):
        n = len(specs)
        for i, (lf, rf, rb) in enumerate(specs):
            self.op("pe", (lambda lf=lf, rf=rf, i=i: self.nc.tensor.matmul(bank_ap(), lf(), rf(), start=(i == 0), stop=(i == n - 1))),
                    reads=list(rb) + list(extra_reads), writes=[bankbuf], signal=(i == n - 1))

    def build(self):
        nc = self.nc
        S = self.S
        dt_in = {}

        def din(name, shape):
            dt_in[name] = nc.dram_tensor(name, list(shape), F32, kind="ExternalInput").ap()
            return dt_in[name]
        self.x_d = din("x", (S, D))
        self.norm_mix = din("norm_mix", (DEPTH, D))
        self.w_in = din("w_in", (DEPTH, D, 2560))
        self.b_in = din("b_in", (DEPTH, 2560))
        self.conv_w = din("conv_w", (DEPTH, CK, CW))
        self.conv_b = din("conv_b", (DEPTH, CW))
        self.conv_ln_g = din("conv_ln_g", (DEPTH, CW))
        self.conv_ln_b = din("conv_ln_b", (DEPTH, CW))
        self.conv_out = din("conv_out", (DEPTH, CW, D))
        self.pool_mix = din("pool_mix", (DEPTH, 4, 128, 128))
        self.pool_scale = din("pool_scale", (DEPTH, 512))
        self.pool_out = din("pool_out", (DEPTH, 512, D))
        self.sgu_ln_g = din("sgu_ln_g", (DEPTH, 512))
        self.sgu_ln_b = din("sgu_ln_b", (DEPTH, 512))
        self.sgu_w = din("sgu_w", (DEPTH, 4, 128, 128))
        self.sgu_b = din("sgu_b", (DEPTH, 4, 128))
        self.sgu_out = din("sgu_out", (DEPTH, 512, D))
        self.w_gate = din("w_gate", (DEPTH, D, 3 * D))
        self.b_gate = din("b_gate", (DEPTH, 3 * D))
        self.w_o = din("w_o", (DEPTH, D, D))
        self.norm_ffn = din("norm_ffn", (DEPTH, D))
        self.ffn_w_up = din("ffn_w_up", (1, D, 2 * D_FF))
        self.ffn_w_down = din("ffn_w_down", (1, D_FF, D))
        self.moe_router = din("moe_router", (1, D, NE))
        self.moe_w_up = din("moe_w_up", (1, NE, D, 2 * D_FFE))
        self.moe_w_down = din("moe_w_down", (1, NE, D_FFE, D))
        self.norm_final = din("norm_final", (D,))
        self.out_d = nc.dram_tensor("out", [S, D], F32, kind="ExternalOutput").ap()

        self.planning = True
        self.tile_idx = 0
        self.tile_program(0)
        self.planning = False
        self.pack_items()
        self.stream = nc.dram_tensor("wstream", [self.nchunk, 128, CHUNK], BF16).ap()

        with contextlib.ExitStack() as st:
            self.setup_tracker(st)

            def sb(name, shape, dt):
                return st.enter_context(nc.sbuf_tensor(name, list(shape), dt))
            self.X = sb("X", (128, 4, 2, 512), F32)
            self.XB = [[Buf("x%d%d" % (tb, dh)) for dh in range(2)] for tb in range(4)]
            self.HT = sb("HT", (128, 8, 512), BF16)
            self.HTB = [Buf("hT%d" % tb) for tb in range(4)]
            self.HC = [sb("HC%d" % l, (128, 4, HALO_C + T), BF16) for l in range(DEPTH)]
            self.HCB = [[Buf() for c in range(4)] for l in range(DEPTH)]
            self.HCH = [[Buf() for c in range(4)] for l in range(DEPTH)]
            self.PP = [sb("PP%d" % l, (128, 4, HALO_P + T), F32) for l in range(DEPTH)]
            self.PPB = [[Buf() for c in range(4)] for l in range(DEPTH)]
            self.PPH = [[Buf() for c in range(4)] for l in range(DEPTH)]
            self.PS1 = sb("PS1", (128, HALO_P + T + 1), F32)
            self.PS2 = sb("PS2", (128, HALO_P + T + 1), F32)
            self.PS1B, self.PS2B = Buf(), Buf()
            self.CT = sb("CT", (128, 4, 512), BF16)
            self.CTB = [Buf() for _ in range(4)]
            self.YP = sb("YP", (128, 4, 512), BF16)
            self.YPB = [Buf() for _ in range(4)]
            self.YS = sb("YS", (128, 4, 512), BF16)
            self.YSB = [Buf() for _ in range(4)]
            self.DT, self.DTB = self.YS, self.YSB
            self.HID = sb("HID", (128, 2, 8, 512), BF16)
            self.HIDB = [[Buf() for _ in range(8)] for _ in range(2)]
            self.VL, self.VLB = self.HID[:, 1, 0:4, :], self.HIDB[1][0:4]
            self.UT, self.UTB = self.HID[:, 1, 4:8, :], self.HIDB[1][4:8]
            self.A32 = sb("A32", (128, NARENA, 512), F32)
            self.AB = [Buf("ar%d" % i) for i in range(NARENA)]
            self.RING = sb("RING", (128, NSLOT, CHUNK), BF16)
            self.slotbuf = [Buf("slot%d" % i) for i in range(NSLOT)]
            self.SM = sb("SM", (128, 32, 16), F32)
            self.SMB = [Buf() for _ in range(32)]
            self.sm_next = 0
            self.GATE = sb("GATE", (128, 4, 8), F32)
            self.GATEB = [Buf() for _ in range(4)]
            self.ident = sb("ident", (128, 128), F32)
            self.identb = sb("identb", (128, 128), BF16)
            self.onesm = sb("onesm", (128, 128), F32)
            self.onesrow = sb("onesrow", (1, 128), BF16)
            self.bsrow = sb("bsrow", (1, DEPTH, 4, 512), BF16)
            self.COLS = sb("COLS", (128, DEPTH, 76), F32)
            self.bin_col = self.COLS[:, :, 0:20]
            self.bgate_col = self.COLS[:, :, 20:44]
            self.convb_col = self.COLS[:, :, 44:48]
            self.clng_col = self.COLS[:, :, 48:52]
            self.clnb_col = self.COLS[:, :, 52:56]
            self.pscale_col = self.COLS[:, :, 56:60]
            self.nmix_col = self.COLS[:, :, 60:68]
            self.nffn_col = self.COLS[:, :, 68:76]
            self.cw_col = sb("cw_col", (128, DEPTH, 4, CK), F32)
            self.sgug_bc = sb("sgug_bc", (128, DEPTH, 512), F32)
            self.sgub_bc = sb("sgub_bc", (128, DEPTH, 512), F32)
            self.bv_bc = sb("bv_bc", (128, DEPTH, 512), F32)
            self.nfin_bc = sb("nfin_bc", (128, 2, 512), F32)
            self.wsT = sb("wsT", (128, DEPTH, 4, 128), BF16)
            self.wmix = sb("wmix", (128, DEPTH, 4, 128), BF16)
            self.R32 = sb("R32", (128, 8, NE), F32)
            self.icnt = sb("icnt", (128, 4, 16), F32)
            self.CONST = Buf("const")
            self.CONSTP = Buf("constp")
            self.CONSTQ = Buf("constq")
            self.CONSTC = Buf("constc")
            self.PS = st.enter_context(nc.psum_tensor("PS", [128, 8, 512], F32))
            self.PSB = [Buf("ps%d" % i) for i in range(8)]
            self.ps_next = 0
            self.ar_next = 0
            for i in range(NSLOT):
                self.newsem("slot%d" % i)
                self.newsem("slotc%d" % i)
                self.newsem("wb%d" % i)
            self.wbbuf = {ch: Buf("wb%d" % ch) for ch in range(self.nchunk)}
            for tb in range(4):
                self.newsem("xin%d" % tb)
                self.newsem("out%d" % tb)
            self.newsem("cst")
            self.newsem("cst2")
            self.newsem("cstq")
            self.groups = []
            for it in self.items:
                if it.group not in self.groups:
                    self.groups.append(it.group)
            self.gbuf = {}
            self.hbuf = {}
            self.newsem("hs0")
            self.newsem("hs1")
            for g in self.groups:
                self.hbuf[g] = Buf("h_" + g)

            self.prologue()
            self.ring_cur = 0
            self.ring_issued = 0
            self.load_x(0)
            for ti in range(self.nt):
                self.tile_idx = ti
                self.item_pos = 0
                self.tile_program(ti)
            for tb in range(4):
                nc.sync.wait_ge(self.sems["out%d" % tb], self.cnt["out%d" % tb])
        return nc

    def cdma(self, out, in_, slow=True, q="pool"):
        if q == "pool":
            self.dma(q, out, in_, "cstq", writes=[self.CONSTQ], serialize=False, slow=slow)
        else:
            self.dma(q, out, in_, "cst", writes=[self.CONST], serialize=False, slow=slow)

    def prologue(self):
        nc = self.nc
        C = self.CONSTP
        self.op("pool", lambda: nc.gpsimd.memset(self.ident[:], 1.0), writes=[C])
        self.op("pool", lambda: nc.gpsimd.affine_select(out=self.ident[:], in_=self.ident[:], pattern=[[1, 128]],
                                                        compare_op=ALU.is_equal, fill=0.0, base=0, channel_multiplier=-1),
                reads=[C], writes=[C])
        self.op("pool", lambda: nc.gpsimd.tensor_copy(out=self.identb[:], in_=self.ident[:]), reads=[C], writes=[C])
        self.op("pool", lambda: nc.gpsimd.memset(self.onesm[:], 1.0 / CW), writes=[C])
        self.op("pool", lambda: nc.gpsimd.memset(self.onesrow[:], 1.0), writes=[C])
        for l in range(DEPTH):
            self.op("pool", lambda l=l: nc.gpsimd.memset(self.HC[l][:, :, 0:HALO_C], 0.0), writes=[b for b in self.HCH[l]])
            self.op("pool", lambda l=l: nc.gpsimd.memset(self.PP[l][:, :, 0:HALO_P], 0.0), writes=[b for b in self.PPH[l]])
        for g, w in enumerate(POOL_W):
            self.op("pool", lambda g=g, w=w: nc.gpsimd.memset(self.icnt[:, g, :], 1.0 / w), writes=[C])
            for t in range(w - 1):
                self.op("pool", lambda g=g, t=t: nc.gpsimd.memset(self.icnt[:, g, t:t + 1], 1.0 / (t + 1)), writes=[C])

        specs = [(self.b_in, 20), (self.b_gate, 24), (self.conv_b, 4), (self.conv_ln_g, 4), (self.conv_ln_b, 4),
                 (self.pool_scale, 4), (self.norm_mix, 8), (self.norm_ffn, 8)]
        stg, stw = [], []
        for l in range(DEPTH):
            a = self.arena(1)
            stg.append(a)
            r0 = 0
            for i, (src, n) in enumerate(specs):
                self.dma("act", self.A32[r0:r0 + n, a, 0:128], src[l].rearrange("(c p) -> c p", p=128), "cst2",
                         writes=[self.AB[a]], serialize=False, accum=(i > 0))
                r0 += n
            assert r0 == 76
            a2 = self.arena(1)
            stw.append(a2)
            self.dma("act", self.A32[0:CK, a2, 0:CW], self.conv_w[l], "cst2", writes=[self.AB[a2]], serialize=False)
        allst = [self.AB[i] for i in stg + stw]
        for b in allst:
            b.w = {"cst2": self.cnt["cst2"]}
        for l in range(DEPTH):
            pb = self.psum()
            self.op("pe", lambda l=l, pb=pb: nc.tensor.transpose(self.PS[:, pb, 0:76], self.A32[0:76, stg[l], 0:128], self.ident[0:76, 0:76]),
                    reads=allst + [C], writes=[self.PSB[pb]])
            self.op("dve", lambda l=l, pb=pb: nc.vector.tensor_copy(out=self.COLS[:, l, :], in_=self.PS[:, pb, 0:76]),
                    reads=[self.PSB[pb]], writes=[self.CONSTC])
            pb2 = self.psum()
            for c in range(4):
                self.op("pe", lambda l=l, pb2=pb2, c=c: nc.tensor.transpose(
                    self.PS[:, pb2, c * 32:c * 32 + CK], self.A32[0:CK, stw[l], c * 128:(c + 1) * 128], self.ident[0:CK, 0:CK]),
                    reads=allst + [C], writes=[self.PSB[pb2]], signal=(c == 3))
            self.op("dve", lambda l=l, pb2=pb2: nc.vector.tensor_copy(
                out=self.cw_col[:, l, :, :], in_=self.PS[:, pb2, 0:128].rearrange("p (c k) -> p c k", k=32)[:, :, 0:CK]),
                reads=[self.PSB[pb2]], writes=[self.CONSTC])
        for l in range(DEPTH):
            self.cdma(self.sgug_bc[:, l, :], self.sgu_ln_g[l:l + 1, :].partition_broadcast(128), q="act")
            self.cdma(self.sgub_bc[:, l, :], self.sgu_ln_b[l:l + 1, :].partition_broadcast(128), q="act")
            self.cdma(self.bv_bc[:, l, :], self.b_in[l:l + 1, 2048:2560].partition_broadcast(128), q="act")
            for g in range(4):
                self.cdma(self.wmix[:, l, g, :], self.pool_mix[l, g], q="pool", slow=False)
            for tb in range(4):
                self.cdma(self.bsrow[0:1, l, :, tb * 128:(tb + 1) * 128], self.sgu_b[l:l + 1, :, :], q="pool", slow=False)
        self.cdma(self.nfin_bc[:].rearrange("p a b -> p (a b)"), self.norm_final.rearrange("(o d) -> o d", o=1).partition_broadcast(128), q="act")
        self.cdma(self.R32[:], self.moe_router[0].rearrange("(kc p) e -> p kc e", p=128), q="act")
        for l in range(DEPTH):
            for h in range(4):
                a = self.arena(1)
                self.dma("act", self.A32[:, a, 0:128], self.sgu_w[l, h], "cst2", writes=[self.AB[a]], serialize=True)
                pb = self.psum()
                self.op("pe", lambda a=a, pb=pb: nc.tensor.transpose(self.PS[:, pb, 0:128], self.A32[:, a, 0:128], self.ident[:]),
                        reads=[self.AB[a], self.CONST], writes=[self.PSB[pb]])
                a2 = self.arena(1)
                self.op("dve", lambda a2=a2, pb=pb: nc.vector.tensor_copy(out=self.A32[:, a2, 0:128], in_=self.PS[:, pb, 0:128]),
                        reads=[self.PSB[pb]], writes=[self.AB[a2]])
                self.op("pool", lambda a2=a2, l=l, h=h: nc.gpsimd.affine_select(
                    out=self.wsT[:, l, h, :], in_=self.A32[:, a2, 0:128], pattern=[[1, 128]], compare_op=ALU.is_ge,
                    fill=0.0, base=0, channel_multiplier=-1), reads=[self.AB[a2]], writes=[C])


    def build_diags(self):
        nc = self.nc
        for it in self.items:
            if it.pieces is not None:
                continue
            dst_full = self.stream[it.chunk, :, it.off:it.off + it.R * it.W].rearrange("p (r w) -> p r w", w=it.W)
            l, c = int(it.name[4]), int(it.name[6])
            on_dve = (l == 0 and c < 2)
            e = "dve" if on_dve else "pool"
            hs = 0 if on_dve else 1
            eng = nc.vector if on_dve else nc.gpsimd
            for k in range(CK):
                self.op(e, lambda l=l, c=c, k=k, hs=hs, eng=eng: eng.tensor_scalar(
                    out=self.HID[:, hs, k // 4, (k % 4) * 128:(k % 4 + 1) * 128], in0=self.ident[:],
                    scalar1=self.cw_col[:, l, c, k:k + 1], scalar2=None, op0=ALU.mult),
                    reads=[self.CONST], writes=self.HIDB[hs])
            src = self.HID[:, hs, :, :].rearrange("p a b -> p (a b)")[:, 0:CK * 128].rearrange("p (r w) -> p r w", w=128)
            self.dma("sp", dst_full, src, "hs%d" % hs, reads=self.HIDB[hs], writes=[self.hbuf[it.group]], serialize=True,
                     accum=bool(self.hbuf[it.group].w))

    def load_x(self, ti):
        for tb in range(4):
            r0 = ti * T + tb * 128
            self.dma("act", self.X[:, tb, :, :], self.x_d[r0:r0 + 128, :].rearrange("p (a b) -> p a b", a=2),
                     "xin%d" % tb, writes=self.XB[tb])

    def tile_program(self, ti):
        for l in range(DEPTH):
            self.cur_group = "L%dmix" % l
            self.mixer(l, ti)
            if self.upto == "mix%d" % l:
                break
            if l % 2 == 0:
                self.cur_group = "L%dffn" % l
                self.norm_to_hT("nffn_col", l, router=False)
                self.ffn("ffn", D_FF // 128, self.ffn_w_up[0], self.ffn_w_down[0], D_FF, None)
            else:
                self.norm_to_hT("nffn_col", l, router=True)
                for e in range(self.nexp):
                    self.cur_group = "E%d" % e
                    self.ffn("e%d" % e, D_FFE // 128, self.moe_w_up[0, e], self.moe_w_down[0, e], D_FFE, e)
            if self.upto == "ffn%d" % l:
                break
        self.final_norm(ti, plain=(self.upto != "all"))
        if not self.planning and ti + 1 < self.nt:
            self.load_x(ti + 1)

    def small(self):
        i = self.sm_next
        self.sm_next = (i + 1) % 32
        return i

    def row_rstd(self, src_aps, src_bufs):
        nc = self.nc
        n = len(src_aps)
        s = self.small()
        for i, (ap, b) in enumerate(zip(src_aps, src_bufs)):
            self.op("dve", lambda ap=ap, i=i, s=s: nc.vector.bn_stats(out=self.SM[:, s, i * 6:(i + 1) * 6], in_=ap()),
                    reads=[b], writes=[self.SMB[s]])
        s2 = self.small()
        self.op("dve", lambda s=s, s2=s2: nc.vector.bn_aggr(out=self.SM[:, s2, 0:2], in_=self.SM[:, s, 0:6 * n]),
                reads=[self.SMB[s]], writes=[self.SMB[s2]])
        return s2

    def rsqrt_eps(self, ap_fn, bufs):
        nc = self.nc
        self.op("dve", lambda: nc.vector.tensor_scalar(out=ap_fn(), in0=ap_fn(), scalar1=EPS, scalar2=None, op0=ALU.add),
                reads=bufs, writes=bufs)
        self.op("act", lambda: nc.scalar.activation(out=ap_fn(), in_=ap_fn(), func=AF.Sqrt), reads=bufs, writes=bufs)
        self.op("dve", lambda: nc.vector.reciprocal(out=ap_fn(), in_=ap_fn()), reads=bufs, writes=bufs)

    def rstd4(self, aps_fn, bufs_fn, use_ms):
        nc = self.nc
        sN = self.small()
        nb = self.SMB[sN]
        for tb in range(4):
            aps = aps_fn(tb)
            bufs = bufs_fn(tb)
            s = self.small()
            for i, ap in enumerate(aps):
                self.op("dve", lambda ap=ap, i=i, s=s: nc.vector.bn_stats(out=self.SM[:, s, i * 6:(i + 1) * 6], in_=ap()),
                        reads=bufs, writes=[self.SMB[s]])
            n = len(aps)
            self.op("dve", lambda s=s, tb=tb, n=n: nc.vector.bn_aggr(out=self.SM[:, sN, tb * 2:tb * 2 + 2], in_=self.SM[:, s, 0:6 * n]),
                    reads=[self.SMB[s]], writes=[nb])
        mv = lambda: self.SM[:, sN, 0:8].rearrange("p (b t) -> p b t", t=2)
        r = lambda: self.SM[:, sN, 8:12]
        if use_ms:
            self.op("dve", lambda: nc.vector.tensor_tensor(out=r(), in0=mv()[:, :, 0], in1=mv()[:, :, 0], op=ALU.mult), reads=[nb], writes=[nb])
            self.op("dve", lambda: nc.vector.tensor_tensor(out=r(), in0=r(), in1=mv()[:, :, 1], op=ALU.add), reads=[nb], writes=[nb])
        else:
            self.op("dve", lambda: nc.vector.tensor_copy(out=r(), in_=mv()[:, :, 1]), reads=[nb], writes=[nb])
        self.rsqrt_eps(r, [nb])
        return sN

    def norm_to_hT(self, gcol, l, router):
        nc = self.nc
        if self.planning:
            return
        gcol = getattr(self, gcol)
        sN = self.rstd4(lambda tb: [(lambda tb=tb, dh=dh: self.X[:, tb, dh, :]) for dh in range(2)], lambda tb: self.XB[tb], True)
        nb = self.SMB[sN]
        hn = []
        if not router:
            for tb in range(4):
                a = self.arena(1)
                hn.append(a)
                self.op("act", lambda a=a, tb=tb: nc.scalar.activation(
                    out=self.A32[:, a, :].bitcast(BF16)[:, 0:1024].rearrange("p (a b) -> p a b", a=2), in_=self.X[:, tb, :, :],
                    func=AF.Copy, scale=self.SM[:, sN, 8 + tb:9 + tb]),
                    reads=self.XB[tb] + [nb], writes=[self.AB[a]])
            for tb in range(4):
                a = hn[tb]
                pb = self.psum()
                for kc in range(8):
                    self.op("pe", lambda a=a, kc=kc, pb=pb: nc.tensor.transpose(
                        self.PS[:, pb, :].bitcast(BF16)[:, kc * 128:(kc + 1) * 128],
                        self.A32[:, a, :].bitcast(BF16)[:, kc * 128:(kc + 1) * 128], self.identb[:]),
                        reads=[self.AB[a], self.CONST], writes=[self.PSB[pb]], signal=(kc == 7))
                self.op("dve", lambda pb=pb, tb=tb: nc.vector.tensor_tensor(
                    out=self.HT[:, :, tb * 128:(tb + 1) * 128],
                    in0=self.PS[:, pb, :].bitcast(BF16)[:, 0:1024].rearrange("p (a b) -> p a b", a=8),
                    in1=gcol[:, l, :].unsqueeze(2).broadcast_to([128, 8, 128]), op=ALU.mult),
                    reads=[self.PSB[pb], self.CONST], writes=[self.HTB[tb]])
            return
        for tb in range(4):
            a = self.arena(2)
            hn.append(a)
            self.op("act", lambda a=a, tb=tb: nc.scalar.activation(
                out=self.A32[:, a:a + 2, :], in_=self.X[:, tb, :, :], func=AF.Copy, scale=self.SM[:, sN, 8 + tb:9 + tb]),
                reads=self.XB[tb] + [nb], writes=[self.AB[a], self.AB[a + 1]])
        for tb in range(4):
            a = hn[tb]
            banks = [self.psum(), self.psum()]
            for kc in range(8):
                pb = banks[kc // 4]
                j = kc % 4
                self.op("pe", lambda a=a, kc=kc, pb=pb, j=j: nc.tensor.transpose(
                    self.PS[:, pb, j * 128:(j + 1) * 128], self.A32[:, a + kc // 4, j * 128:(j + 1) * 128], self.ident[:]),
                    reads=[self.AB[a + kc // 4], self.CONST], writes=[self.PSB[pb]], signal=(j == 3))
            if router:
                h32 = self.arena(2)
            for kc in range(8):
                pb = banks[kc // 4]
                j = kc % 4
                if router:
                    dst = (lambda h32=h32, kc=kc, j=j: self.A32[:, h32 + kc // 4, j * 128:(j + 1) * 128])
                    dbuf = [self.AB[h32 + kc // 4]]
                else:
                    dst = (lambda kc=kc, tb=tb: self.HT[:, kc, tb * 128:(tb + 1) * 128])
                    dbuf = [self.HTB[tb]]
                if kc % 2 == 0:
                    self.op("dve", lambda dst=dst, pb=pb, j=j, kc=kc: nc.vector.tensor_scalar(
                        out=dst(), in0=self.PS[:, pb, j * 128:(j + 1) * 128], scalar1=gcol[:, l, kc:kc + 1], scalar2=None,
                        op0=ALU.mult), reads=[self.PSB[pb], self.CONST], writes=dbuf)
                else:
                    self.op("act", lambda dst=dst, pb=pb, j=j, kc=kc: nc.scalar.activation(
                        out=dst(), in_=self.PS[:, pb, j * 128:(j + 1) * 128], func=AF.Copy, scale=gcol[:, l, kc:kc + 1]),
                        reads=[self.PSB[pb], self.CONST], writes=dbuf)
            if router:
                for half in range(2):
                    self.op("act" if half else "dve", (lambda h32=h32, half=half, tb=tb: (
                        nc.scalar.copy(out=self.HT[:, half * 4:(half + 1) * 4, tb * 128:(tb + 1) * 128],
                                       in_=self.A32[:, h32 + half, :].rearrange("p (a b) -> p a b", a=4)) if half else
                        nc.vector.tensor_copy(out=self.HT[:, half * 4:(half + 1) * 4, tb * 128:(tb + 1) * 128],
                                              in_=self.A32[:, h32 + half, :].rearrange("p (a b) -> p a b", a=4)))),
                        reads=[self.AB[h32 + half]], writes=[self.HTB[tb]])
                self.router(tb, h32)

    def router(self, tb, h32):
        nc = self.nc
        pb = self.psum()
        for kc in range(8):
            self.op("pe", lambda kc=kc, pb=pb, h32=h32: nc.tensor.matmul(
                self.PS[:, pb, 0:NE], self.A32[:, h32 + kc // 4, (kc % 4) * 128:(kc % 4 + 1) * 128], self.R32[:, kc, :],
                start=(kc == 0), stop=(kc == 7)), reads=[self.AB[h32 + kc // 4], self.CONST], writes=[self.PSB[pb]],
                signal=(kc == 7))
        s = self.small()
        sb_ = self.SMB[s]
        SMs = self.SM

        def dv(fn, extra=()):
            self.op("dve", fn, reads=[sb_] + list(extra), writes=[sb_])
        s_lg, s_e1, s_l2, s_e2 = s, self.small(), self.small(), self.small()
        sc = self.small()
        bufs = [self.SMB[i] for i in (s_lg, s_e1, s_l2, s_e2, sc)]

        def dv2(fn, extra=()):
            self.op("dve", fn, reads=bufs + list(extra), writes=bufs)
        dv2(lambda: nc.vector.tensor_copy(out=SMs[:, s_lg, 0:NE], in_=self.PS[:, pb, 0:NE]), [self.PSB[pb]])
        dv2(lambda: nc.vector.reduce_max(out=SMs[:, sc, 0:1], in_=SMs[:, s_lg, 0:NE], axis=AX.X))
        dv2(lambda: nc.vector.tensor_scalar(out=SMs[:, s_e1, 0:NE], in0=SMs[:, s_lg, 0:NE], scalar1=SMs[:, sc, 0:1],
                                            scalar2=None, op0=ALU.is_equal))
        dv2(lambda: nc.vector.scalar_tensor_tensor(out=SMs[:, s_l2, 0:NE], in0=SMs[:, s_e1, 0:NE], scalar=-1e30,
                                                   in1=SMs[:, s_lg, 0:NE], op0=ALU.mult, op1=ALU.add))
        dv2(lambda: nc.vector.reduce_max(out=SMs[:, sc, 1:2], in_=SMs[:, s_l2, 0:NE], axis=AX.X))
        dv2(lambda: nc.vector.tensor_scalar(out=SMs[:, s_e2, 0:NE], in0=SMs[:, s_l2, 0:NE], scalar1=SMs[:, sc, 1:2],
                                            scalar2=None, op0=ALU.is_equal))
        dv2(lambda: nc.vector.tensor_tensor(out=SMs[:, sc, 2:3], in0=SMs[:, sc, 1:2], in1=SMs[:, sc, 0:1], op=ALU.subtract))
        self.op("act", lambda: nc.scalar.activation(out=SMs[:, sc, 3:4], in_=SMs[:, sc, 2:3], func=AF.Exp),
                reads=bufs, writes=bufs)
        dv2(lambda: nc.vector.tensor_scalar(out=SMs[:, sc, 4:5], in0=SMs[:, sc, 3:4], scalar1=1.0, scalar2=None, op0=ALU.add))
        dv2(lambda: nc.vector.reciprocal(out=SMs[:, sc, 5:6], in_=SMs[:, sc, 4:5]))
        dv2(lambda: nc.vector.tensor_tensor(out=SMs[:, sc, 6:7], in0=SMs[:, sc, 3:4], in1=SMs[:, sc, 5:6], op=ALU.mult))
        dv2(lambda: nc.vector.tensor_scalar(out=SMs[:, s_e1, 0:NE], in0=SMs[:, s_e1, 0:NE], scalar1=SMs[:, sc, 5:6],
                                            scalar2=None, op0=ALU.mult))
        self.op("dve", lambda: nc.vector.scalar_tensor_tensor(out=self.GATE[:, tb, :], in0=SMs[:, s_e2, 0:NE],
                                                              scalar=SMs[:, sc, 6:7], in1=SMs[:, s_e1, 0:NE],
                                                              op0=ALU.mult, op1=ALU.add),
                reads=bufs, writes=[self.GATEB[tb]])

    def hT_specs(self, it, r_of_kc, c0, n=128):
        specs = []
        for kc in range(8):
            specs.append(((lambda kc=kc: self.iap(it, r_of_kc(kc), c0, n)),
                          (lambda kc=kc: self.HT[:, kc, :]),
                          [self.ibuf(it)] + self.HTB if not self.planning else []))
        return specs

    def gelu(self, out_fn, in_fn, bias_fn, reads, writes):
        nc = self.nc
        if GELU_FUNC == "native":
            if bias_fn is None:
                self.op("act", lambda: nc.scalar.activation(out=out_fn(), in_=in_fn(), func=AF.Gelu_apprx_tanh),
                        reads=reads, writes=writes)
            else:
                self.op("act", lambda: nc.scalar.activation(out=out_fn(), in_=in_fn(), func=AF.Gelu_apprx_tanh,
                                                            bias=bias_fn(), scale=1.0), reads=reads, writes=writes)
            return
        a = self.arena(2)
        xa = lambda: self.A32[:, a, :]
        ta = lambda: self.A32[:, a + 1, :]
        ab = [self.AB[a], self.AB[a + 1]]
        if bias_fn is None:
            self.op("act", lambda: nc.scalar.copy(out=xa(), in_=in_fn()), reads=reads, writes=ab)
        else:
            self.op("act", lambda: nc.scalar.activation(out=xa(), in_=in_fn(), func=AF.Identity, bias=bias_fn(), scale=1.0),
                    reads=reads, writes=ab)
        self.op("dve", lambda: nc.vector.tensor_tensor(out=ta(), in0=xa(), in1=xa(), op=ALU.mult), reads=ab, writes=ab)
        self.op("dve", lambda: nc.vector.tensor_scalar(out=ta(), in0=ta(), scalar1=0.044715, scalar2=1.0, op0=ALU.mult,
                                                       op1=ALU.add), reads=ab, writes=ab)
        self.op("dve", lambda: nc.vector.tensor_tensor(out=ta(), in0=ta(), in1=xa(), op=ALU.mult), reads=ab, writes=ab)
        self.op("act", lambda: nc.scalar.activation(out=ta(), in_=ta(), func=AF.Sigmoid, scale=1.5957691216057308),
                reads=ab, writes=ab)
        self.op("dve", lambda: nc.vector.tensor_tensor(out=out_fn(), in0=ta(), in1=xa(), op=ALU.mult), reads=ab, writes=writes)

    def mixer(self, l, ti):
        nc = self.nc
        P = self.planning

        def colsrc(w, c0, n):
            return w[:, c0:c0 + n].rearrange("(kc p) j -> p kc j", p=128)
        self.norm_to_hT("nmix_col", l, router=False)
        if not P and ti == 0 and l == 0:
            self.build_diags()
        itA = self.item("win_conv%d" % l, 8, 1024, [(0, 8, 0, 1024, colsrc(self.w_in[l], 0, 1024))])
        for c in range(4):
            ba, bg = self.psum(), self.psum()
            if not P:
                self.mm_group(lambda ba=ba: self.PS[:, ba, :], self.PSB[ba], self.hT_specs(itA, lambda kc: kc, c * 128))
                self.mm_group(lambda bg=bg: self.PS[:, bg, :], self.PSB[bg], self.hT_specs(itA, lambda kc: kc, 512 + c * 128))
                a = self.arena(1)
                self.op("act", lambda a=a, bg=bg, c=c: nc.scalar.activation(
                    out=self.A32[:, a, :], in_=self.PS[:, bg, :], func=AF.Sigmoid, bias=self.bin_col[:, l, 4 + c:5 + c], scale=1.0),
                    reads=[self.PSB[bg], self.CONST], writes=[self.AB[a]])
                self.op("dve", lambda a=a, ba=ba, c=c: nc.vector.scalar_tensor_tensor(
                    out=self.HC[l][:, c, HALO_C:HALO_C + T], in0=self.PS[:, ba, :], scalar=self.bin_col[:, l, c:c + 1],
                    in1=self.A32[:, a, :], op0=ALU.add, op1=ALU.mult),
                    reads=[self.PSB[ba], self.AB[a], self.CONST], writes=[self.HCB[l][c]])
        rc, rq = [], []
        for c in range(4):
            itD = self.item("diag%d_%d" % (l, c), CK, 128, None)
            if P:
                continue
            bc = self.psum()
            specs = []
            for k in range(CK):
                specs.append(((lambda k=k, itD=itD: self.iap(itD, k, 0, 128)),
                              (lambda k=k, c=c: self.HC[l][:, c, k:k + T]),
                              [self.ibuf(itD), self.HCB[l][c], self.HCH[l][c]]))
            self.mm_group(lambda bc=bc: self.PS[:, bc, :], self.PSB[bc], specs)
            a = self.arena(2)
            rc.append(a)
            rq.append(a + 1)
            self.op("act", lambda a=a, bc=bc, c=c: nc.scalar.activation(
                out=self.A32[:, a, :], in_=self.PS[:, bc, :], func=AF.Identity, bias=self.convb_col[:, l, c:c + 1], scale=1.0),
                reads=[self.PSB[bc], self.CONST], writes=[self.AB[a]])
            self.op("act", lambda a=a, bc=bc, c=c: nc.scalar.activation(
                out=self.A32[:, a + 1, :], in_=self.PS[:, bc, :], func=AF.Square, bias=self.convb_col[:, l, c:c + 1], scale=1.0),
                reads=[self.PSB[bc], self.CONST], writes=[self.AB[a + 1]])
            self.op("act", lambda c=c: nc.scalar.copy(out=self.HC[l][:, c, 0:HALO_C], in_=self.HC[l][:, c, T:T + HALO_C]),
                    reads=[self.HCB[l][c]], writes=[self.HCH[l][c]])
        if not P:
            bm, bq = self.psum(), self.psum()
            self.mm_group(lambda: self.PS[:, bm, :], self.PSB[bm],
                          [((lambda: self.onesm[:]), (lambda c=c: self.A32[:, rc[c], :]), [self.AB[rc[c]], self.CONST]) for c in range(4)])
            self.mm_group(lambda: self.PS[:, bq, :], self.PSB[bq],
                          [((lambda: self.onesm[:]), (lambda c=c: self.A32[:, rq[c], :]), [self.AB[rq[c]], self.CONST]) for c in range(4)])
            am = self.arena(2)
            mean = lambda: self.A32[:, am, :]
            rstd = lambda: self.A32[:, am + 1, :]
            mb = [self.AB[am], self.AB[am + 1]]
            self.op("dve", lambda: nc.vector.tensor_copy(out=mean(), in_=self.PS[:, bm, :]), reads=[self.PSB[bm]], writes=[mb[0]])
            self.op("dve", lambda: nc.vector.tensor_tensor(out=rstd(), in0=mean(), in1=mean(), op=ALU.mult), reads=[mb[0]], writes=[mb[1]])
            self.op("dve", lambda: nc.vector.tensor_tensor(out=rstd(), in0=self.PS[:, bq, :], in1=rstd(), op=ALU.subtract),
                    reads=[self.PSB[bq], mb[1]], writes=[mb[1]])
            self.rsqrt_eps(rstd, [mb[1]])
            for c in range(4):
                self.op("dve", lambda c=c: nc.vector.tensor_tensor(out=self.A32[:, rc[c], :], in0=self.A32[:, rc[c], :], in1=mean(), op=ALU.subtract),
                        reads=[self.AB[rc[c]], mb[0]], writes=[self.AB[rc[c]]])
                self.op("dve", lambda c=c: nc.vector.tensor_tensor(out=self.A32[:, rc[c], :], in0=self.A32[:, rc[c], :], in1=rstd(), op=ALU.mult),
                        reads=[self.AB[rc[c]], mb[1]], writes=[self.AB[rc[c]]])
                self.op("act", lambda c=c: nc.scalar.activation(out=self.CT[:, c, :], in_=self.A32[:, rc[c], :], func=AF.Silu,
                                                                bias=self.clnb_col[:, l, c:c + 1], scale=self.clng_col[:, l, c:c + 1]),
                        reads=[self.AB[rc[c]], self.CONST], writes=[self.CTB[c]])
        itP = self.item("win_pool%d" % l, 8, 512, [(0, 8, 0, 512, colsrc(self.w_in[l], 1024, 512))])
        PPl = self.PP[l] if not P else None
        L = HALO_P + T
        if not P:
            for g, w in enumerate(POOL_W):
                pb = self.psum()
                self.mm_group(lambda pb=pb: self.PS[:, pb, :], self.PSB[pb], self.hT_specs(itP, lambda kc: kc, g * 128))
                self.op("act", lambda pb=pb, g=g: nc.scalar.activation(
                    out=PPl[:, g, HALO_P:HALO_P + T], in_=self.PS[:, pb, :], func=AF.Identity, bias=self.bin_col[:, l, 8 + g:9 + g], scale=1.0),
                    reads=[self.PSB[pb], self.CONST], writes=[self.PPB[l][g]])
        itU = self.item("win_u%d" % l, 8, 512, [(0, 8, 0, 512, colsrc(self.w_in[l], 1536, 512))])
        if not P:
            for h in range(4):
                pb = self.psum()
                self.mm_group(lambda pb=pb: self.PS[:, pb, :], self.PSB[pb], self.hT_specs(itU, lambda kc: kc, h * 128))
                self.gelu(lambda h=h: self.UT[:, h, :], lambda pb=pb: self.PS[:, pb, :], lambda h=h: self.bin_col[:, l, 12 + h:13 + h],
                          [self.PSB[pb], self.CONST], [self.UTB[h]])
            for g, w in enumerate(POOL_W):
                src_fn = (lambda g=g: PPl[:, g, :])
                src_b = [self.PPB[l][g], self.PPH[l][g]]
                tmp = [(self.PS1, self.PS1B), (self.PS2, self.PS2B)]
                sh = 1
                lvl = 0
                lo = 0
                while sh < w:
                    dstT, dstB = tmp[lvl % 2]
                    lo2 = lo + sh
                    self.op("dve", lambda src_fn=src_fn, dstT=dstT, lo2=lo2, sh=sh: nc.vector.tensor_tensor(
                        out=dstT[:, lo2:L], in0=src_fn()[:, lo2:L], in1=src_fn()[:, lo2 - sh:L - sh], op=ALU.add),
                        reads=src_b, writes=[dstB])
                    src_fn = (lambda dstT=dstT: dstT[:, 0:L])
                    src_b = [dstB]
                    lo = lo2
                    sh *= 2
                    lvl += 1
                assert lo == w - 1
                self.op("dve", lambda src_fn=src_fn, g=g, w=w: nc.vector.scalar_tensor_tensor(
                    out=self.DT[:, g, :], in0=src_fn()[:, HALO_P:L], scalar=1.0 / w, in1=PPl[:, g, HALO_P:L],
                    op0=ALU.mult, op1=ALU.subtract), reads=src_b + [self.PPB[l][g]], writes=[self.DTB[g]])
                if ti == 0:
                    a = self.arena(1)
                    self.op("dve", lambda src_fn=src_fn, g=g, a=a: nc.vector.tensor_tensor(
                        out=self.A32[:, a, 0:16], in0=src_fn()[:, HALO_P:HALO_P + 16], in1=self.icnt[:, g, :], op=ALU.mult),
                        reads=src_b + [self.CONST], writes=[self.AB[a]])
                    self.op("dve", lambda g=g, a=a: nc.vector.tensor_tensor(
                        out=self.DT[:, g, 0:16], in0=self.A32[:, a, 0:16], in1=PPl[:, g, HALO_P:HALO_P + 16], op=ALU.subtract),
                        reads=[self.AB[a], self.PPB[l][g]], writes=[self.DTB[g]])
                self.op("act", lambda g=g: nc.scalar.copy(out=PPl[:, g, 0:HALO_P], in_=PPl[:, g, T:T + HALO_P]),
                        reads=[self.PPB[l][g]], writes=[self.PPH[l][g]])
        itV = self.item("win_v%d" % l, 8, 512, [(0, 8, 0, 512, colsrc(self.w_in[l], 2048, 512))])
        if not P:
            vas = []
            for tb in range(4):
                pb = self.psum()
                specs = [((lambda kc=kc, tb=tb: self.HT[:, kc, tb * 128:(tb + 1) * 128]),
                          (lambda kc=kc: self.iap(itV, kc, 0, 512)), [self.ibuf(itV)] + self.HTB) for kc in range(8)]
                self.mm_group(lambda pb=pb: self.PS[:, pb, :], self.PSB[pb], specs)
                a = self.arena(1)
                vas.append(a)
                va = (lambda a=a: self.A32[:, a, :])
                vb = [self.AB[a]]
                self.op("dve", lambda pb=pb, va=va: nc.vector.tensor_tensor(out=va(), in0=self.PS[:, pb, :], in1=self.bv_bc[:, l, :], op=ALU.add),
                        reads=[self.PSB[pb], self.CONST], writes=vb)
                self.gelu(va, va, None, vb, vb)
            for g in range(4):
                pb2 = self.psum()
                self.mm_group(lambda pb2=pb2: self.PS[:, pb2, :], self.PSB[pb2],
                              [((lambda g=g: self.wmix[:, l, g, :]), (lambda g=g: self.DT[:, g, :]), [self.CONST, self.DTB[g]])])
                self.op("act", lambda pb2=pb2, g=g: nc.scalar.activation(
                    out=self.YP[:, g, :], in_=self.PS[:, pb2, :], func=AF.Copy, scale=self.pscale_col[:, l, g:g + 1]),
                    reads=[self.PSB[pb2], self.CONST], writes=[self.YPB[g]])
            sN = self.rstd4(lambda tb: [(lambda tb=tb: self.A32[:, vas[tb], :])], lambda tb: [self.AB[vas[tb]]], False)
            nb = self.SMB[sN]
            for tb in range(4):
                a = vas[tb]
                va = (lambda a=a: self.A32[:, a, :])
                vb = [self.AB[a]]
                self.op("dve", lambda va=va, tb=tb: nc.vector.tensor_scalar(
                    out=va(), in0=va(), scalar1=self.SM[:, sN, 2 * tb:2 * tb + 1], scalar2=self.SM[:, sN, 8 + tb:9 + tb],
                    op0=ALU.subtract, op1=ALU.mult), reads=vb + [nb], writes=vb)
                self.op("dve", lambda va=va: nc.vector.tensor_tensor(out=va(), in0=va(), in1=self.sgug_bc[:, l, :], op=ALU.mult),
                        reads=vb + [self.CONST], writes=vb)
                self.op("dve", lambda va=va, tb=tb: nc.vector.tensor_tensor(out=self.VL[:, tb, :], in0=va(), in1=self.sgub_bc[:, l, :], op=ALU.add),
                        reads=vb + [self.CONST], writes=[self.VLB[tb]])
            for h in range(4):
                pb = self.psum()
                self.op("pe", lambda pb=pb, h=h: nc.tensor.matmul(
                    self.PS[:, pb, :], self.onesrow[0:1, :], self.bsrow[0:1, l, h, :], start=True, stop=False),
                    reads=[self.CONST], writes=[self.PSB[pb]], signal=False)
                for tb in range(4):
                    self.op("pe", lambda pb=pb, tb=tb, h=h: nc.tensor.matmul(
                        self.PS[:, pb, tb * 128:(tb + 1) * 128], self.VL[:, tb, h * 128:(h + 1) * 128], self.wsT[:, l, h, :],
                        start=False, stop=(tb == 3)), reads=[self.VLB[tb], self.CONST], writes=[self.PSB[pb]], signal=(tb == 3))
                self.op("dve", lambda pb=pb, h=h: nc.vector.tensor_tensor(out=self.YS[:, h, :], in0=self.PS[:, pb, :], in1=self.UT[:, h, :], op=ALU.mult),
                        reads=[self.PSB[pb], self.UTB[h]], writes=[self.YSB[h]])
        outs_w = (self.conv_out[l], self.pool_out[l], self.sgu_out[l])
        for dc in range(8):
            itG = self.item("gate%d_%d" % (l, dc), 24, 128,
                            [(i * 8, 8, 0, 128, colsrc(self.w_gate[l], i * 1024 + dc * 128, 128)) for i in range(3)])
            if P:
                self.item("outs%d_%d" % (l, dc), 12, 128,
                          [(i * 4, 4, 0, 128, colsrc(outs_w[i], dc * 128, 128)) for i in range(3)])
                continue
            ga = []
            for i in range(3):
                pb = self.psum()
                self.mm_group(lambda pb=pb: self.PS[:, pb, :], self.PSB[pb], self.hT_specs(itG, lambda kc, i=i: i * 8 + kc, 0))
                a = self.arena(1)
                ga.append(a)
                self.op("act", lambda pb=pb, a=a, i=i: nc.scalar.activation(
                    out=self.A32[:, a, :], in_=self.PS[:, pb, :], func=AF.Sigmoid, bias=self.bgate_col[:, l, i * 8 + dc:i * 8 + dc + 1], scale=1.0),
                    reads=[self.PSB[pb], self.CONST], writes=[self.AB[a]])
            itO = self.item("outs%d_%d" % (l, dc))
            srcs = ((self.CT, self.CTB), (self.YP, self.YPB), (self.YS, self.YSB))
            for i in range(3):
                pb = self.psum()
                sT, sB = srcs[i]
                specs = [((lambda c=c, i=i: self.iap(itO, i * 4 + c, 0, 128)), (lambda c=c, sT=sT: sT[:, c, :]), [self.ibuf(itO), sB[c]])
                         for c in range(4)]
                self.mm_group(lambda pb=pb: self.PS[:, pb, :], self.PSB[pb], specs)
                a = ga[i]
                self.op("dve", lambda pb=pb, a=a: nc.vector.tensor_tensor(out=self.A32[:, a, :], in0=self.A32[:, a, :], in1=self.PS[:, pb, :], op=ALU.mult),
                        reads=[self.PSB[pb], self.AB[a]], writes=[self.AB[a]])
            self.op("dve", lambda ga=ga: nc.vector.tensor_tensor(out=self.A32[:, ga[0], :], in0=self.A32[:, ga[0], :], in1=self.A32[:, ga[1], :], op=ALU.add),
                    reads=[self.AB[ga[0]], self.AB[ga[1]]], writes=[self.AB[ga[0]]])
            self.op("dve", lambda ga=ga, dc=dc: nc.vector.tensor_tensor(out=self.HID[:, 0, dc, :], in0=self.A32[:, ga[0], :], in1=self.A32[:, ga[2], :], op=ALU.add),
                    reads=[self.AB[ga[0]], self.AB[ga[2]]], writes=[self.HIDB[0][dc]])
        for dh in range(2):
            itW = self.item("wo%d_%d" % (l, dh), 8, 512, [(0, 8, 0, 512, colsrc(self.w_o[l], dh * 512, 512))])
            if P:
                continue
            for tb in range(4):
                pb = self.psum()
                specs = [((lambda kc=kc, tb=tb: self.HID[:, 0, kc, tb * 128:(tb + 1) * 128]),
                          (lambda kc=kc, itW=itW: self.iap(itW, kc, 0, 512)), [self.ibuf(itW), self.HIDB[0][kc]]) for kc in range(8)]
                self.mm_group(lambda pb=pb: self.PS[:, pb, :], self.PSB[pb], specs)
                self.op("dve", lambda pb=pb, tb=tb, dh=dh: nc.vector.tensor_tensor(
                    out=self.X[:, tb, dh, :], in0=self.X[:, tb, dh, :], in1=self.PS[:, pb, :], op=ALU.add),
                    reads=[self.PSB[pb], self.XB[tb][dh]], writes=[self.XB[tb][dh]])

    def ffn(self, pfx, nf, w_up, w_down, F, expert):
        nc = self.nc
        P = self.planning
        segs = []
        f = 0
        while f < nf:
            n = min(8, nf - f)
            segs.append((f, n))
            f += n
        ups = {}

        def up_item(q):
            nq = min(4, nf - q * 4)
            pieces = [(0, 8, 0, nq * 128, w_up[:, q * 512:q * 512 + nq * 128].rearrange("(kc p) j -> p kc j", p=128)),
                      (0, 8, nq * 128, nq * 128, w_up[:, F + q * 512:F + q * 512 + nq * 128].rearrange("(kc p) j -> p kc j", p=128))]
            return self.item("%s_up%d" % (pfx, q), 8, 2 * nq * 128, pieces)

        def do_up(si):
            f0, n = segs[si]
            hb = si % 2
            for jj in range(n):
                fch = f0 + jj
                q = fch // 4
                if (fch % 4) == 0:
                    ups[q] = up_item(q)
                if P:
                    continue
                it = ups[q]
                j4 = fch % 4
                ba, bb = self.psum(), self.psum()
                self.mm_group(lambda ba=ba: self.PS[:, ba, :], self.PSB[ba], self.hT_specs(it, lambda kc: kc, j4 * 128))
                self.mm_group(lambda bb=bb: self.PS[:, bb, :], self.PSB[bb], self.hT_specs(it, lambda kc: kc, it.W // 2 + j4 * 128))
                a = self.arena(1)
                self.op("act", lambda a=a, ba=ba: nc.scalar.activation(out=self.A32[:, a, :], in_=self.PS[:, ba, :], func=AF.Silu),
                        reads=[self.PSB[ba]], writes=[self.AB[a]])
                self.op("dve", lambda a=a, bb=bb, hb=hb, jj=jj: nc.vector.tensor_tensor(
                    out=self.HID[:, hb, jj, :], in0=self.A32[:, a, :], in1=self.PS[:, bb, :], op=ALU.mult),
                    reads=[self.AB[a], self.PSB[bb]], writes=[self.HIDB[hb][jj]])

        def do_down(si):
            f0, n = segs[si]
            hb = si % 2
            it = self.item("%s_down%d" % (pfx, si), n, 1024,
                           [(0, n, 0, 1024, w_down[f0 * 128:(f0 + n) * 128, :].rearrange("(r p) j -> p r j", p=128))])
            if P:
                return
            for tb in range(4):
                for dh in range(2):
                    pb = self.psum()
                    specs = [((lambda jj=jj, tb=tb: self.HID[:, hb, jj, tb * 128:(tb + 1) * 128]),
                              (lambda jj=jj, dh=dh: self.iap(it, jj, dh * 512, 512)), [self.ibuf(it), self.HIDB[hb][jj]]) for jj in range(n)]
                    self.mm_group(lambda pb=pb: self.PS[:, pb, :], self.PSB[pb], specs)
                    if expert is None:
                        self.op("dve", lambda pb=pb, tb=tb, dh=dh: nc.vector.tensor_tensor(
                            out=self.X[:, tb, dh, :], in0=self.X[:, tb, dh, :], in1=self.PS[:, pb, :], op=ALU.add),
                            reads=[self.PSB[pb], self.XB[tb][dh]], writes=[self.XB[tb][dh]])
                    else:
                        self.op("dve", lambda pb=pb, tb=tb, dh=dh: nc.vector.scalar_tensor_tensor(
                            out=self.X[:, tb, dh, :], in0=self.PS[:, pb, :], scalar=self.GATE[:, tb, expert:expert + 1],
                            in1=self.X[:, tb, dh, :], op0=ALU.mult, op1=ALU.add),
                            reads=[self.PSB[pb], self.XB[tb][dh], self.GATEB[tb]], writes=[self.XB[tb][dh]])
        do_up(0)
        for si in range(len(segs)):
            if si + 1 < len(segs):
                do_up(si + 1)
            do_down(si)

    def final_norm(self, ti, plain=False):
        nc = self.nc
        if self.planning:
            return
        if not plain:
            sN = self.rstd4(lambda tb: [(lambda tb=tb, dh=dh: self.X[:, tb, dh, :]) for dh in range(2)], lambda tb: self.XB[tb], True)
            nb = self.SMB[sN]
        for tb in range(4):
            a = self.arena(2)
            ab = [self.AB[a], self.AB[a + 1]]
            if plain:
                self.op("dve", lambda a=a, tb=tb: nc.vector.tensor_copy(out=self.A32[:, a:a + 2, :], in_=self.X[:, tb, :, :]),
                        reads=self.XB[tb], writes=ab)
            else:
                self.op("dve", lambda a=a, tb=tb: nc.vector.scalar_tensor_tensor(
                    out=self.A32[:, a:a + 2, :], in0=self.X[:, tb, :, :], scalar=self.SM[:, sN, 8 + tb:9 + tb], in1=self.nfin_bc[:],
                    op0=ALU.mult, op1=ALU.mult), reads=self.XB[tb] + [nb, self.CONST], writes=ab)
            r0 = ti * T + tb * 128
            self.dma("act", self.out_d[r0:r0 + 128, :].rearrange("p (a b) -> p a b", a=2), self.A32[:, a:a + 2, :],
                     "out%d" % tb, reads=ab)


_CACHE = {}
WNAMES = ["norm_mix", "w_in", "b_in", "conv_w", "conv_b", "conv_ln_g", "conv_ln_b", "conv_out", "pool_mix", "pool_scale",
          "pool_out", "sgu_ln_g", "sgu_ln_b", "sgu_w", "sgu_b", "sgu_out", "w_gate", "b_gate", "w_o", "norm_ffn",
          "ffn_w_up", "ffn_w_down", "moe_router", "moe_w_up", "moe_w_down", "norm_final"]


def run(inputs, nt, ncores, nexp=NE, upto="all", trace=False):
    key = (nt, nexp, upto)
    if key not in _CACHE:
        _CACHE[key] = Builder(nt, nt * T, nexp, upto).build()
    nc = _CACHE[key]
    x = np.asarray(inputs["x"], dtype=np.float32)
    shared = {k: np.ascontiguousarray(np.asarray(inputs[k], dtype=np.float32)) for k in WNAMES}
    in_maps = []
    for b in range(ncores):
        m = dict(shared)
        m["x"] = np.ascontiguousarray(x[b, :nt * T])
        in_maps.append(m)
    res = run_bass_kernel_spmd(nc, in_maps, core_ids=list(range(ncores)), trace=trace)
    out = np.stack([np.asarray(r["out"]) for r in res.results], axis=0)
    return out, res


def kernel(**inputs):
    out, _ = run(inputs, nt=8, ncores=8)
    return out.astype(np.float32)
```

```python
import contextlib
import numpy as np
import concourse.bass as bass
import concourse.mybir as mybir
from concourse.bass_utils import run_bass_kernel_spmd

F32 = mybir.dt.float32
BF16 = mybir.dt.bfloat16
AF = mybir.ActivationFunctionType
ALU = mybir.AluOpType
AX = mybir.AxisListType

D = 1024
T = 512
DEPTH = 2
CW = 512
CK = 31
HALO_C = CK - 1
POOL_W = (2, 4, 8, 16)
HALO_P = 15
D_FF = 2816
NE = 8
D_FFE = 3584
EPS = 1e-6
CHUNK = 8192
NSLOT = 4
NARENA = 12
GELU_FUNC = "native"


class Buf:
    __slots__ = ("w", "r", "name")

    def __init__(self, name=""):
        self.w = {}
        self.r = {}
        self.name = name


class Item:
    __slots__ = ("name", "R", "W", "pieces", "chunk", "off", "group")

    def __init__(self, name, R, W, pieces, group):
        self.name, self.R, self.W, self.pieces, self.group = name, R, W, pieces, group
        self.chunk = None
        self.off = None


class Builder:
    def __init__(self, nt, seq_total, nexp=NE, upto="all"):
        self.nt = nt
        self.S = nt * T
        self.nexp = nexp
        self.upto = upto
        self.nc = bass.Bass("TRN2", target_bir_lowering=False)
        self.planning = False
        self.items = []
        self.item_by_name = {}
        self.item_pos = 0
        self.cur_group = None
        self.ps_next = 0
        self.ar_next = 0
        self.sm_next = 0
        self.diag_i = 0

    def setup_tracker(self, stack):
        nc = self.nc
        self.eng = {"pe": nc.tensor, "act": nc.scalar, "dve": nc.vector, "pool": nc.gpsimd, "sp": nc.sync}
        self.sems = {}
        self.cnt = {}
        self.seen = {e: {} for e in self.eng}
        self.stack = stack
        for e in self.eng:
            self.newsem(e)

    def newsem(self, key):
        self.sems[key] = self.stack.enter_context(self.nc.semaphore("s_" + key))
        self.cnt[key] = 0

    def _waits(self, e, reads, writes, extra=()):
        deps = {}

        def add(tok):
            if tok is None:
                return
            k, v = tok
            if deps.get(k, 0) < v:
                deps[k] = v
        for b in reads:
            for k, v in b.w.items():
                add((k, v))
        for b in writes:
            for k, v in b.w.items():
                add((k, v))
            for k, v in b.r.items():
                add((k, v))
        for tok in extra:
            add(tok)
        for k, v in deps.items():
            if k == e:
                if e == "pe" or e == "sp":
                    continue
            if self.seen[e].get(k, 0) >= v:
                continue
            self.eng[e].wait_ge(self.sems[k], v)
            self.seen[e][k] = v

    def op(self, e, fn, reads=(), writes=(), signal=True):
        if self.planning:
            return None
        if self.CONST in reads:
            reads = list(reads) + [self.CONSTP, self.CONSTQ, self.CONSTC]
        self._waits(e, reads, writes)
        ins = fn()
        tok = (e, self.cnt[e] + 1)
        if signal:
            ins.then_inc(self.sems[e], 1)
            self.cnt[e] += 1
        for b in writes:
            b.w = {tok[0]: tok[1]}
            b.r = {}
        for b in reads:
            if b.r.get(e, 0) < tok[1]:
                b.r[e] = tok[1]
        return ins

    def dma(self, q, out, in_, key, reads=(), writes=(), serialize=True, slow=False, accum=False, extra_toks=()):
        if self.planning:
            return
        extra = [(key, self.cnt[key])] if (serialize and self.cnt[key] > 0) else []
        extra = extra + list(extra_toks)
        self._waits(q, reads, writes if not accum else (), extra)
        self.cnt[key] += 16
        tok = (key, self.cnt[key])
        if slow:
            self.eng[q].dma_start(out=out, in_=in_, allow_slow_non_contiguous=True).then_inc(self.sems[key], 16)
        else:
            self.eng[q].dma_start(out=out, in_=in_).then_inc(self.sems[key], 16)
        for b in writes:
            if accum:
                b.w[key] = tok[1]
            else:
                b.w = {key: tok[1]}
                b.r = {}
        for b in reads:
            if b.r.get(key, 0) < tok[1]:
                b.r[key] = tok[1]

    def item(self, name, R=None, W=None, pieces=None):
        if self.planning:
            it = Item(name, R, W, pieces, self.cur_group)
            self.items.append(it)
            self.item_by_name[name] = it
            return it
        it = self.item_by_name[name]
        gchunk = self.tile_idx * self.nchunk + it.chunk
        assert gchunk >= self.ring_cur, (name, gchunk, self.ring_cur)
        if gchunk > self.ring_cur or self.ring_issued == 0:
            self.ring_cur = gchunk
            lim = min(gchunk - 1 + NSLOT, self.nt * self.nchunk - 1)
            while self.ring_issued <= lim:
                g = self.ring_issued
                ch = g % self.nchunk
                slot = g % NSLOT
                used = self.chunk_used[ch]
                sbuf_ = self.slotbuf[slot]
                if g < self.nchunk:
                    pre = list(sbuf_.w.items()) + list(sbuf_.r.items())
                    first = True
                    for cit in self.chunk_items[ch]:
                        sz = cit.R * cit.W
                        if cit.pieces is None:
                            self.dma("sp", self.RING[:, slot, cit.off:cit.off + sz], self.stream[ch, :, cit.off:cit.off + sz],
                                     "slot%d" % slot, reads=[self.hbuf[cit.group]], writes=[sbuf_], accum=not first, extra_toks=pre)
                            first = False
                        else:
                            view = self.RING[:, slot, cit.off:cit.off + sz].rearrange("p (r w) -> p r w", w=cit.W)
                            for (r0, nr, c0, ncw, src) in cit.pieces:
                                self.dma("pool", view[:, r0:r0 + nr, c0:c0 + ncw], src, "slotc%d" % slot, writes=[sbuf_],
                                         serialize=False, accum=not first, extra_toks=pre)
                                first = False
                    for cit in self.chunk_items[ch]:
                        if cit.pieces is not None:
                            sz = cit.R * cit.W
                            self.dma("sp", self.stream[ch, :, cit.off:cit.off + sz], self.RING[:, slot, cit.off:cit.off + sz],
                                     "wb%d" % slot, reads=[sbuf_], writes=[self.wbbuf[ch]], serialize=False, accum=True)
                else:
                    self.dma("sp", self.RING[:, slot, 0:used], self.stream[ch, :, 0:used], "slot%d" % slot,
                             reads=[self.wbbuf[ch]], writes=[sbuf_])
                self.ring_issued += 1
        return it

    def iap(self, it, r, c0, n):
        gchunk = self.tile_idx * self.nchunk + it.chunk
        assert gchunk >= self.ring_cur, ("stale ring item", it.name)
        slot = gchunk % NSLOT
        o = it.off + r * it.W + c0
        return self.RING[:, slot, o:o + n]

    def ibuf(self, it):
        gchunk = self.tile_idx * self.nchunk + it.chunk
        return self.slotbuf[gchunk % NSLOT]

    def pack_items(self):
        ch, off = 0, 0
        self.chunk_groups = {}
        self.chunk_items = {}
        self.chunk_used = {}
        for it in self.items:
            sz = it.R * it.W
            assert sz <= CHUNK
            if off + sz > CHUNK:
                ch += 1
                off = 0
            it.chunk, it.off = ch, off
            off += sz
            self.chunk_groups.setdefault(ch, set()).add(it.group)
            self.chunk_items.setdefault(ch, []).append(it)
            self.chunk_used[ch] = off
        self.nchunk = ch + 1

    def psum(self):
        i = self.ps_next
        self.ps_next = (i + 1) % 8
        return i

    def arena(self, n=1):
        i = self.ar_next
        if i + n > NARENA:
            i = 0
        self.ar_next = (i + n) % NARENA
        return i

    def mm_group(self, bank_ap, bankbuf, specs, extra_reads=()):
        n = len(specs)
        for i, (lf, rf, rb) in enumerate(specs):
            self.op("pe", (lambda lf=lf, rf=rf, i=i: self.nc.tensor.matmul(bank_ap(), lf(), rf(), start=(i == 0), stop=(i == n - 1))),
                    reads=list(rb) + list(extra_reads), writes=[bankbuf], signal=(i == n - 1))

    def build(self):
        nc = self.nc
        S = self.S
        dt_in = {}

        def din(name, shape):
            dt_in[name] = nc.dram_tensor(name, list(shape), F32, kind="ExternalInput").ap()
            return dt_in[name]
        self.x_d = din("x", (S, D))
        self.norm_mix = din("norm_mix", (DEPTH, D))
        self.w_in = din("w_in", (DEPTH, D, 2560))
        self.b_in = din("b_in", (DEPTH, 2560))
        self.conv_w = din("conv_w", (DEPTH, CK, CW))
        self.conv_b = din("conv_b", (DEPTH, CW))
        self.conv_ln_g = din("conv_ln_g", (DEPTH, CW))
        self.conv_ln_b = din("conv_ln_b", (DEPTH, CW))
        self.conv_out = din("conv_out", (DEPTH, CW, D))
        self.pool_mix = din("pool_mix", (DEPTH, 4, 128, 128))
        self.pool_scale = din("pool_scale", (DEPTH, 512))
        self.pool_out = din("pool_out", (DEPTH, 512, D))
        self.sgu_ln_g = din("sgu_ln_g", (DEPTH, 512))
        self.sgu_ln_b = din("sgu_ln_b", (DEPTH, 512))
        self.sgu_w = din("sgu_w", (DEPTH, 4, 128, 128))
        self.sgu_b = din("sgu_b", (DEPTH, 4, 128))
        self.sgu_out = din("sgu_out", (DEPTH, 512, D))
        self.w_gate = din("w_gate", (DEPTH, D, 3 * D))
        self.b_gate = din("b_gate", (DEPTH, 3 * D))
        self.w_o = din("w_o", (DEPTH, D, D))
        self.norm_ffn = din("norm_ffn", (DEPTH, D))
        self.ffn_w_up = din("ffn_w_up", (1, D, 2 * D_FF))
        self.ffn_w_down = din("ffn_w_down", (1, D_FF, D))
        self.moe_router = din("moe_router", (1, D, NE))
        self.moe_w_up = din("moe_w_up", (1, NE, D, 2 * D_FFE))
        self.moe_w_down = din("moe_w_down", (1, NE, D_FFE, D))
        self.norm_final = din("norm_final", (D,))
        self.out_d = nc.dram_tensor("out", [S, D], F32, kind="ExternalOutput").ap()

        self.planning = True
        self.tile_idx = 0
        self.tile_program(0)
        self.planning = False
        self.pack_items()
        self.stream = nc.dram_tensor("wstream", [self.nchunk, 128, CHUNK], BF16).ap()

        with contextlib.ExitStack() as st:
            self.setup_tracker(st)

            def sb(name, shape, dt):
                return st.enter_context(nc.sbuf_tensor(name, list(shape), dt))
            self.X = sb("X", (128, 4, 2, 512), F32)
            self.XB = [[Buf("x%d%d" % (tb, dh)) for dh in range(2)] for tb in range(4)]
            self.HT = sb("HT", (128, 8, 512), BF16)
            self.HTB = [Buf("hT%d" % tb) for tb in range(4)]
            self.HC = [sb("HC%d" % l, (128, 4, HALO_C + T), BF16) for l in range(DEPTH)]
            self.HCB = [[Buf() for c in range(4)] for l in range(DEPTH)]
            self.HCH = [[Buf() for c in range(4)] for l in range(DEPTH)]
            self.PP = [sb("PP%d" % l, (128, 4, HALO_P + T), F32) for l in range(DEPTH)]
            self.PPB = [[Buf() for c in range(4)] for l in range(DEPTH)]
            self.PPH = [[Buf() for c in range(4)] for l in range(DEPTH)]
            self.PS1 = sb("PS1", (128, HALO_P + T + 1), F32)
            self.PS2 = sb("PS2", (128, HALO_P + T + 1), F32)
            self.PS1B, self.PS2B = Buf(), Buf()
            self.CT = sb("CT", (128, 4, 512), BF16)
            self.CTB = [Buf() for _ in range(4)]
            self.YP = sb("YP", (128, 4, 512), BF16)
            self.YPB = [Buf() for _ in range(4)]
            self.YS = sb("YS", (128, 4, 512), BF16)
            self.YSB = [Buf() for _ in range(4)]
            self.DT, self.DTB = self.YS, self.YSB
            self.HID = sb("HID", (128, 2, 8, 512), BF16)
            self.HIDB = [[Buf() for _ in range(8)] for _ in range(2)]
            self.VL, self.VLB = self.HID[:, 1, 0:4, :], self.HIDB[1][0:4]
            self.UT, self.UTB = self.HID[:, 1, 4:8, :], self.HIDB[1][4:8]
            self.A32 = sb("A32", (128, NARENA, 512), F32)
            self.AB = [Buf("ar%d" % i) for i in range(NARENA)]
            self.RING = sb("RING", (128, NSLOT, CHUNK), BF16)
            self.slotbuf = [Buf("slot%d" % i) for i in range(NSLOT)]
            self.SM = sb("SM", (128, 32, 16), F32)
            self.SMB = [Buf() for _ in range(32)]
            self.sm_next = 0
            self.GATE = sb("GATE", (128, 4, 8), F32)
            self.GATEB = [Buf() for _ in range(4)]
            self.ident = sb("ident", (128, 128), F32)
            self.identb = sb("identb", (128, 128), BF16)
            self.onesm = sb("onesm", (128, 128), F32)
            self.onesrow = sb("onesrow", (1, 128), BF16)
            self.bsrow = sb("bsrow", (1, DEPTH, 4, 512), BF16)
            self.COLS = sb("COLS", (128, DEPTH, 76), F32)
            self.bin_col = self.COLS[:, :, 0:20]
            self.bgate_col = self.COLS[:, :, 20:44]
            self.convb_col = self.COLS[:, :, 44:48]
            self.clng_col = self.COLS[:, :, 48:52]
            self.clnb_col = self.COLS[:, :, 52:56]
            self.pscale_col = self.COLS[:, :, 56:60]
            self.nmix_col = self.COLS[:, :, 60:68]
            self.nffn_col = self.COLS[:, :, 68:76]
            self.cw_col = sb("cw_col", (128, DEPTH, 4, CK), F32)
            self.sgug_bc = sb("sgug_bc", (128, DEPTH, 512), F32)
            self.sgub_bc = sb("sgub_bc", (128, DEPTH, 512), F32)
            self.bv_bc = sb("bv_bc", (128, DEPTH, 512), F32)
            self.nfin_bc = sb("nfin_bc", (128, 2, 512), F32)
            self.wsT = sb("wsT", (128, DEPTH, 4, 128), BF16)
            self.wmix = sb("wmix", (128, DEPTH, 4, 128), BF16)
            self.R32 = sb("R32", (128, 8, NE), F32)
            self.icnt = sb("icnt", (128, 4, 16), F32)
            self.CONST = Buf("const")
            self.CONSTP = Buf("constp")
            self.CONSTQ = Buf("constq")
            self.CONSTC = Buf("constc")
            self.PS = st.enter_context(nc.psum_tensor("PS", [128, 8, 512], F32))
            self.PSB = [Buf("ps%d" % i) for i in range(8)]
            self.ps_next = 0
            self.ar_next = 0
            for i in range(NSLOT):
                self.newsem("slot%d" % i)
                self.newsem("slotc%d" % i)
                self.newsem("wb%d" % i)
            self.wbbuf = {ch: Buf("wb%d" % ch) for ch in range(self.nchunk)}
            for tb in range(4):
                self.newsem("xin%d" % tb)
                self.newsem("out%d" % tb)
            self.newsem("cst")
            self.newsem("cst2")
            self.newsem("cstq")
            self.groups = []
            for it in self.items:
                if it.group not in self.groups:
                    self.groups.append(it.group)
            self.gbuf = {}
            self.hbuf = {}
            self.newsem("hs0")
            self.newsem("hs1")
            for g in self.groups:
                self.hbuf[g] = Buf("h_" + g)

            self.prologue()
            self.ring_cur = 0
            self.ring_issued = 0
            self.load_x(0)
            for ti in range(self.nt):
                self.tile_idx = ti
                self.item_pos = 0
                self.tile_program(ti)
            for tb in range(4):
                nc.sync.wait_ge(self.sems["out%d" % tb], self.cnt["out%d" % tb])
        return nc

    def cdma(self, out, in_, slow=True, q="pool"):
        if q == "pool":
            self.dma(q, out, in_, "cstq", writes=[self.CONSTQ], serialize=False, slow=slow)
        else:
            self.dma(q, out, in_, "cst", writes=[self.CONST], serialize=False, slow=slow)

    def prologue(self):
        nc = self.nc
        C = self.CONSTP
        self.op("pool", lambda: nc.gpsimd.memset(self.ident[:], 1.0), writes=[C])
        self.op("pool", lambda: nc.gpsimd.affine_select(out=self.ident[:], in_=self.ident[:], pattern=[[1, 128]],
                                                        compare_op=ALU.is_equal, fill=0.0, base=0, channel_multiplier=-1),
                reads=[C], writes=[C])
        self.op("pool", lambda: nc.gpsimd.tensor_copy(out=self.identb[:], in_=self.ident[:]), reads=[C], writes=[C])
        self.op("pool", lambda: nc.gpsimd.memset(self.onesm[:], 1.0 / CW), writes=[C])
        self.op("pool", lambda: nc.gpsimd.memset(self.onesrow[:], 1.0), writes=[C])
        for l in range(DEPTH):
            self.op("pool", lambda l=l: nc.gpsimd.memset(self.HC[l][:, :, 0:HALO_C], 0.0), writes=[b for b in self.HCH[l]])
            self.op("pool", lambda l=l: nc.gpsimd.memset(self.PP[l][:, :, 0:HALO_P], 0.0), writes=[b for b in self.PPH[l]])
        for g, w in enumerate(POOL_W):
            self.op("pool", lambda g=g, w=w: nc.gpsimd.memset(self.icnt[:, g, :], 1.0 / w), writes=[C])
            for t in range(w - 1):
                self.op("pool", lambda g=g, t=t: nc.gpsimd.memset(self.icnt[:, g, t:t + 1], 1.0 / (t + 1)), writes=[C])

        specs = [(self.b_in, 20), (self.b_gate, 24), (self.conv_b, 4), (self.conv_ln_g, 4), (self.conv_ln_b, 4),
                 (self.pool_scale, 4), (self.norm_mix, 8), (self.norm_ffn, 8)]
        stg, stw = [], []
        for l in range(DEPTH):
            a = self.arena(1)
            stg.append(a)
            r0 = 0
            for i, (src, n) in enumerate(specs):
                self.dma("act", self.A32[r0:r0 + n, a, 0:128], src[l].rearrange("(c p) -> c p", p=128), "cst2",
                         writes=[self.AB[a]], serialize=False, accum=(i > 0))
                r0 += n
            assert r0 == 76
            a2 = self.arena(1)
            stw.append(a2)
            self.dma("act", self.A32[0:CK, a2, 0:CW], self.conv_w[l], "cst2", writes=[self.AB[a2]], serialize=False)
        allst = [self.AB[i] for i in stg + stw]
        for b in allst:
            b.w = {"cst2": self.cnt["cst2"]}
        for l in range(DEPTH):
            pb = self.psum()
            self.op("pe", lambda l=l, pb=pb: nc.tensor.transpose(self.PS[:, pb, 0:76], self.A32[0:76, stg[l], 0:128], self.ident[0:76, 0:76]),
                    reads=allst + [C], writes=[self.PSB[pb]])
            self.op("dve", lambda l=l, pb=pb: nc.vector.tensor_copy(out=self.COLS[:, l, :], in_=self.PS[:, pb, 0:76]),
                    reads=[self.PSB[pb]], writes=[self.CONSTC])
            pb2 = self.psum()
            for c in range(4):
                self.op("pe", lambda l=l, pb2=pb2, c=c: nc.tensor.transpose(
                    self.PS[:, pb2, c * 32:c * 32 + CK], self.A32[0:CK, stw[l], c * 128:(c + 1) * 128], self.ident[0:CK, 0:CK]),
                    reads=allst + [C], writes=[self.PSB[pb2]], signal=(c == 3))
            self.op("dve", lambda l=l, pb2=pb2: nc.vector.tensor_copy(
                out=self.cw_col[:, l, :, :], in_=self.PS[:, pb2, 0:128].rearrange("p (c k) -> p c k", k=32)[:, :, 0:CK]),
                reads=[self.PSB[pb2]], writes=[self.CONSTC])
        for l in range(DEPTH):
            self.cdma(self.sgug_bc[:, l, :], self.sgu_ln_g[l:l + 1, :].partition_broadcast(128), q="act")
            self.cdma(self.sgub_bc[:, l, :], self.sgu_ln_b[l:l + 1, :].partition_broadcast(128), q="act")
            self.cdma(self.bv_bc[:, l, :], self.b_in[l:l + 1, 2048:2560].partition_broadcast(128), q="act")
            for g in range(4):
                self.cdma(self.wmix[:, l, g, :], self.pool_mix[l, g], q="pool", slow=False)
            for tb in range(4):
                self.cdma(self.bsrow[0:1, l, :, tb * 128:(tb + 1) * 128], self.sgu_b[l:l + 1, :, :], q="pool", slow=False)
        self.cdma(self.nfin_bc[:].rearrange("p a b -> p (a b)"), self.norm_final.rearrange("(o d) -> o d", o=1).partition_broadcast(128), q="act")
        self.cdma(self.R32[:], self.moe_router[0].rearrange("(kc p) e -> p kc e", p=128), q="act")
        for l in range(DEPTH):
            for h in range(4):
                a = self.arena(1)
                self.dma("act", self.A32[:, a, 0:128], self.sgu_w[l, h], "cst2", writes=[self.AB[a]], serialize=True)
                pb = self.psum()
                self.op("pe", lambda a=a, pb=pb: nc.tensor.transpose(self.PS[:, pb, 0:128], self.A32[:, a, 0:128], self.ident[:]),
                        reads=[self.AB[a], self.CONST], writes=[self.PSB[pb]])
                a2 = self.arena(1)
                self.op("dve", lambda a2=a2, pb=pb: nc.vector.tensor_copy(out=self.A32[:, a2, 0:128], in_=self.PS[:, pb, 0:128]),
                        reads=[self.PSB[pb]], writes=[self.AB[a2]])
                self.op("pool", lambda a2=a2, l=l, h=h: nc.gpsimd.affine_select(
                    out=self.wsT[:, l, h, :], in_=self.A32[:, a2, 0:128], pattern=[[1, 128]], compare_op=ALU.is_ge,
                    fill=0.0, base=0, channel_multiplier=-1), reads=[self.AB[a2]], writes=[C])


    def build_diags(self):
        nc = self.nc
        for it in self.items:
            if it.pieces is not None:
                continue
            dst_full = self.stream[it.chunk, :, it.off:it.off + it.R * it.W].rearrange("p (r w) -> p r w", w=it.W)
            l, c = int(it.name[4]), int(it.name[6])
            hs = self.diag_i % 2
            self.diag_i += 1
            self.op("dve", lambda l=l, c=c, hs=hs: nc.vector.tensor_tensor(
                out=self.HID[:, hs, :, :].rearrange("p a b -> p (a b)")[:, 0:CK * 128].rearrange("p (k w) -> p k w", w=128),
                in0=self.ident[:].unsqueeze(1).broadcast_to([128, CK, 128]),
                in1=self.cw_col[:, l, c, :].unsqueeze(2).broadcast_to([128, CK, 128]), op=ALU.mult),
                reads=[self.CONST], writes=self.HIDB[hs])
            src = self.HID[:, hs, :, :].rearrange("p a b -> p (a b)")[:, 0:CK * 128].rearrange("p (r w) -> p r w", w=128)
            self.dma("sp", dst_full, src, "hs%d" % hs, reads=self.HIDB[hs], writes=[self.hbuf[it.group]], serialize=True,
                     accum=bool(self.hbuf[it.group].w))

    def load_x(self, ti):
        for tb in range(4):
            r0 = ti * T + tb * 128
            self.dma("act", self.X[:, tb, :, :], self.x_d[r0:r0 + 128, :].rearrange("p (a b) -> p a b", a=2),
                     "xin%d" % tb, writes=self.XB[tb])

    def tile_program(self, ti):
        for l in range(DEPTH):
            self.cur_group = "L%dmix" % l
            self.mixer(l, ti)
            if self.upto == "mix%d" % l:
                break
            if l % 2 == 0:
                self.cur_group = "L%dffn" % l
                self.norm_to_hT("nffn_col", l, router=False)
                self.ffn("ffn", D_FF // 128, self.ffn_w_up[0], self.ffn_w_down[0], D_FF, None)
            else:
                self.norm_to_hT("nffn_col", l, router=True)
                for e in range(self.nexp):
                    self.cur_group = "E%d" % e
                    self.ffn("e%d" % e, D_FFE // 128, self.moe_w_up[0, e], self.moe_w_down[0, e], D_FFE, e)
            if self.upto == "ffn%d" % l:
                break
        self.final_norm(ti, plain=(self.upto != "all"))
        if not self.planning and ti + 1 < self.nt:
            self.load_x(ti + 1)

    def small(self):
        i = self.sm_next
        self.sm_next = (i + 1) % 32
        return i

    def row_rstd(self, src_aps, src_bufs):
        nc = self.nc
        n = len(src_aps)
        s = self.small()
        for i, (ap, b) in enumerate(zip(src_aps, src_bufs)):
            self.op("dve", lambda ap=ap, i=i, s=s: nc.vector.bn_stats(out=self.SM[:, s, i * 6:(i + 1) * 6], in_=ap()),
                    reads=[b], writes=[self.SMB[s]])
        s2 = self.small()
        self.op("dve", lambda s=s, s2=s2: nc.vector.bn_aggr(out=self.SM[:, s2, 0:2], in_=self.SM[:, s, 0:6 * n]),
                reads=[self.SMB[s]], writes=[self.SMB[s2]])
        return s2

    def rsqrt_eps(self, ap_fn, bufs):
        nc = self.nc
        self.op("dve", lambda: nc.vector.tensor_scalar(out=ap_fn(), in0=ap_fn(), scalar1=EPS, scalar2=None, op0=ALU.add),
                reads=bufs, writes=bufs)
        self.op("act", lambda: nc.scalar.activation(out=ap_fn(), in_=ap_fn(), func=AF.Sqrt), reads=bufs, writes=bufs)
        self.op("dve", lambda: nc.vector.reciprocal(out=ap_fn(), in_=ap_fn()), reads=bufs, writes=bufs)

    def rstd4(self, aps_fn, bufs_fn, use_ms):
        nc = self.nc
        sN = self.small()
        nb = self.SMB[sN]
        for tb in range(4):
            aps = aps_fn(tb)
            bufs = bufs_fn(tb)
            s = self.small()
            for i, ap in enumerate(aps):
                self.op("dve", lambda ap=ap, i=i, s=s: nc.vector.bn_stats(out=self.SM[:, s, i * 6:(i + 1) * 6], in_=ap()),
                        reads=bufs, writes=[self.SMB[s]])
            n = len(aps)
            self.op("dve", lambda s=s, tb=tb, n=n: nc.vector.bn_aggr(out=self.SM[:, sN, tb * 2:tb * 2 + 2], in_=self.SM[:, s, 0:6 * n]),
                    reads=[self.SMB[s]], writes=[nb])
        mv = lambda: self.SM[:, sN, 0:8].rearrange("p (b t) -> p b t", t=2)
        r = lambda: self.SM[:, sN, 8:12]
        if use_ms:
            self.op("dve", lambda: nc.vector.tensor_tensor(out=r(), in0=mv()[:, :, 0], in1=mv()[:, :, 0], op=ALU.mult), reads=[nb], writes=[nb])
            self.op("dve", lambda: nc.vector.tensor_tensor(out=r(), in0=r(), in1=mv()[:, :, 1], op=ALU.add), reads=[nb], writes=[nb])
        else:
            self.op("dve", lambda: nc.vector.tensor_copy(out=r(), in_=mv()[:, :, 1]), reads=[nb], writes=[nb])
        self.rsqrt_eps(r, [nb])
        return sN

    def norm_to_hT(self, gcol, l, router):
        nc = self.nc
        if self.planning:
            return
        gcol = getattr(self, gcol)
        sN = self.rstd4(lambda tb: [(lambda tb=tb, dh=dh: self.X[:, tb, dh, :]) for dh in range(2)], lambda tb: self.XB[tb], True)
        nb = self.SMB[sN]
        hn = []
        if not router:
            for tb in range(4):
                a = self.arena(1)
                hn.append(a)
                self.op("act", lambda a=a, tb=tb: nc.scalar.activation(
                    out=self.A32[:, a, :].bitcast(BF16)[:, 0:1024].rearrange("p (a b) -> p a b", a=2), in_=self.X[:, tb, :, :],
                    func=AF.Copy, scale=self.SM[:, sN, 8 + tb:9 + tb]),
                    reads=self.XB[tb] + [nb], writes=[self.AB[a]])
            for tb in range(4):
                a = hn[tb]
                pb = self.psum()
                for kc in range(8):
                    self.op("pe", lambda a=a, kc=kc, pb=pb: nc.tensor.transpose(
                        self.PS[:, pb, :].bitcast(BF16)[:, kc * 128:(kc + 1) * 128],
                        self.A32[:, a, :].bitcast(BF16)[:, kc * 128:(kc + 1) * 128], self.identb[:]),
                        reads=[self.AB[a], self.CONST], writes=[self.PSB[pb]], signal=(kc == 7))
                self.op("dve", lambda pb=pb, tb=tb: nc.vector.tensor_tensor(
                    out=self.HT[:, :, tb * 128:(tb + 1) * 128],
                    in0=self.PS[:, pb, :].bitcast(BF16)[:, 0:1024].rearrange("p (a b) -> p a b", a=8),
                    in1=gcol[:, l, :].unsqueeze(2).broadcast_to([128, 8, 128]), op=ALU.mult),
                    reads=[self.PSB[pb], self.CONST], writes=[self.HTB[tb]])
            return
        for tb in range(4):
            a = self.arena(2)
            hn.append(a)
            self.op("act", lambda a=a, tb=tb: nc.scalar.activation(
                out=self.A32[:, a:a + 2, :], in_=self.X[:, tb, :, :], func=AF.Copy, scale=self.SM[:, sN, 8 + tb:9 + tb]),
                reads=self.XB[tb] + [nb], writes=[self.AB[a], self.AB[a + 1]])
        for tb in range(4):
            a = hn[tb]
            banks = [self.psum(), self.psum()]
            for kc in range(8):
                pb = banks[kc // 4]
                j = kc % 4
                self.op("pe", lambda a=a, kc=kc, pb=pb, j=j: nc.tensor.transpose(
                    self.PS[:, pb, j * 128:(j + 1) * 128], self.A32[:, a + kc // 4, j * 128:(j + 1) * 128], self.ident[:]),
                    reads=[self.AB[a + kc // 4], self.CONST], writes=[self.PSB[pb]], signal=(j == 3))
            if router:
                h32 = self.arena(2)
            for kc in range(8):
                pb = banks[kc // 4]
                j = kc % 4
                if router:
                    dst = (lambda h32=h32, kc=kc, j=j: self.A32[:, h32 + kc // 4, j * 128:(j + 1) * 128])
                    dbuf = [self.AB[h32 + kc // 4]]
                else:
                    dst = (lambda kc=kc, tb=tb: self.HT[:, kc, tb * 128:(tb + 1) * 128])
                    dbuf = [self.HTB[tb]]
                if kc % 2 == 0:
                    self.op("dve", lambda dst=dst, pb=pb, j=j, kc=kc: nc.vector.tensor_scalar(
                        out=dst(), in0=self.PS[:, pb, j * 128:(j + 1) * 128], scalar1=gcol[:, l, kc:kc + 1], scalar2=None,
                        op0=ALU.mult), reads=[self.PSB[pb], self.CONST], writes=dbuf)
                else:
                    self.op("act", lambda dst=dst, pb=pb, j=j, kc=kc: nc.scalar.activation(
                        out=dst(), in_=self.PS[:, pb, j * 128:(j + 1) * 128], func=AF.Copy, scale=gcol[:, l, kc:kc + 1]),
                        reads=[self.PSB[pb], self.CONST], writes=dbuf)
            if router:
                for half in range(2):
                    self.op("act" if half else "dve", (lambda h32=h32, half=half, tb=tb: (
                        nc.scalar.copy(out=self.HT[:, half * 4:(half + 1) * 4, tb * 128:(tb + 1) * 128],
                                       in_=self.A32[:, h32 + half, :].rearrange("p (a b) -> p a b", a=4)) if half else
                        nc.vector.tensor_copy(out=self.HT[:, half * 4:(half + 1) * 4, tb * 128:(tb + 1) * 128],
                                              in_=self.A32[:, h32 + half, :].rearrange("p (a b) -> p a b", a=4)))),
                        reads=[self.AB[h32 + half]], writes=[self.HTB[tb]])
                self.router(tb, h32)

    def router(self, tb, h32):
        nc = self.nc
        pb = self.psum()
        for kc in range(8):
            self.op("pe", lambda kc=kc, pb=pb, h32=h32: nc.tensor.matmul(
                self.PS[:, pb, 0:NE], self.A32[:, h32 + kc // 4, (kc % 4) * 128:(kc % 4 + 1) * 128], self.R32[:, kc, :],
                start=(kc == 0), stop=(kc == 7)), reads=[self.AB[h32 + kc // 4], self.CONST], writes=[self.PSB[pb]],
                signal=(kc == 7))
        s = self.small()
        sb_ = self.SMB[s]
        SMs = self.SM

        def dv(fn, extra=()):
            self.op("dve", fn, reads=[sb_] + list(extra), writes=[sb_])
        s_lg, s_e1, s_l2, s_e2 = s, self.small(), self.small(), self.small()
        sc = self.small()
        bufs = [self.SMB[i] for i in (s_lg, s_e1, s_l2, s_e2, sc)]

        def dv2(fn, extra=()):
            self.op("dve", fn, reads=bufs + list(extra), writes=bufs)
        dv2(lambda: nc.vector.tensor_copy(out=SMs[:, s_lg, 0:NE], in_=self.PS[:, pb, 0:NE]), [self.PSB[pb]])
        dv2(lambda: nc.vector.reduce_max(out=SMs[:, sc, 0:1], in_=SMs[:, s_lg, 0:NE], axis=AX.X))
        dv2(lambda: nc.vector.tensor_scalar(out=SMs[:, s_e1, 0:NE], in0=SMs[:, s_lg, 0:NE], scalar1=SMs[:, sc, 0:1],
                                            scalar2=None, op0=ALU.is_equal))
        dv2(lambda: nc.vector.scalar_tensor_tensor(out=SMs[:, s_l2, 0:NE], in0=SMs[:, s_e1, 0:NE], scalar=-1e30,
                                                   in1=SMs[:, s_lg, 0:NE], op0=ALU.mult, op1=ALU.add))
        dv2(lambda: nc.vector.reduce_max(out=SMs[:, sc, 1:2], in_=SMs[:, s_l2, 0:NE], axis=AX.X))
        dv2(lambda: nc.vector.tensor_scalar(out=SMs[:, s_e2, 0:NE], in0=SMs[:, s_l2, 0:NE], scalar1=SMs[:, sc, 1:2],
                                            scalar2=None, op0=ALU.is_equal))
        dv2(lambda: nc.vector.tensor_tensor(out=SMs[:, sc, 2:3], in0=SMs[:, sc, 1:2], in1=SMs[:, sc, 0:1], op=ALU.subtract))
        self.op("act", lambda: nc.scalar.activation(out=SMs[:, sc, 3:4], in_=SMs[:, sc, 2:3], func=AF.Exp),
                reads=bufs, writes=bufs)
        dv2(lambda: nc.vector.tensor_scalar(out=SMs[:, sc, 4:5], in0=SMs[:, sc, 3:4], scalar1=1.0, scalar2=None, op0=ALU.add))
        dv2(lambda: nc.vector.reciprocal(out=SMs[:, sc, 5:6], in_=SMs[:, sc, 4:5]))
        dv2(lambda: nc.vector.tensor_tensor(out=SMs[:, sc, 6:7], in0=SMs[:, sc, 3:4], in1=SMs[:, sc, 5:6], op=ALU.mult))
        dv2(lambda: nc.vector.tensor_scalar(out=SMs[:, s_e1, 0:NE], in0=SMs[:, s_e1, 0:NE], scalar1=SMs[:, sc, 5:6],
                                            scalar2=None, op0=ALU.mult))
        self.op("dve", lambda: nc.vector.scalar_tensor_tensor(out=self.GATE[:, tb, :], in0=SMs[:, s_e2, 0:NE],
                                                              scalar=SMs[:, sc, 6:7], in1=SMs[:, s_e1, 0:NE],
                                                              op0=ALU.mult, op1=ALU.add),
                reads=bufs, writes=[self.GATEB[tb]])

    def hT_specs(self, it, r_of_kc, c0, n=128):
        specs = []
        for kc in range(8):
            specs.append(((lambda kc=kc: self.iap(it, r_of_kc(kc), c0, n)),
                          (lambda kc=kc: self.HT[:, kc, :]),
                          [self.ibuf(it)] + self.HTB if not self.planning else []))
        return specs

    def gelu(self, out_fn, in_fn, bias_fn, reads, writes):
        nc = self.nc
        if GELU_FUNC == "native":
            if bias_fn is None:
                self.op("act", lambda: nc.scalar.activation(out=out_fn(), in_=in_fn(), func=AF.Gelu_apprx_tanh),
                        reads=reads, writes=writes)
            else:
                self.op("act", lambda: nc.scalar.activation(out=out_fn(), in_=in_fn(), func=AF.Gelu_apprx_tanh,
                                                            bias=bias_fn(), scale=1.0), reads=reads, writes=writes)
            return
        a = self.arena(2)
        xa = lambda: self.A32[:, a, :]
        ta = lambda: self.A32[:, a + 1, :]
        ab = [self.AB[a], self.AB[a + 1]]
        if bias_fn is None:
            self.op("act", lambda: nc.scalar.copy(out=xa(), in_=in_fn()), reads=reads, writes=ab)
        else:
            self.op("act", lambda: nc.scalar.activation(out=xa(), in_=in_fn(), func=AF.Identity, bias=bias_fn(), scale=1.0),
                    reads=reads, writes=ab)
        self.op("dve", lambda: nc.vector.tensor_tensor(out=ta(), in0=xa(), in1=xa(), op=ALU.mult), reads=ab, writes=ab)
        self.op("dve", lambda: nc.vector.tensor_scalar(out=ta(), in0=ta(), scalar1=0.044715, scalar2=1.0, op0=ALU.mult,
                                                       op1=ALU.add), reads=ab, writes=ab)
        self.op("dve", lambda: nc.vector.tensor_tensor(out=ta(), in0=ta(), in1=xa(), op=ALU.mult), reads=ab, writes=ab)
        self.op("act", lambda: nc.scalar.activation(out=ta(), in_=ta(), func=AF.Sigmoid, scale=1.5957691216057308),
                reads=ab, writes=ab)
        self.op("dve", lambda: nc.vector.tensor_tensor(out=out_fn(), in0=ta(), in1=xa(), op=ALU.mult), reads=ab, writes=writes)

    def mixer(self, l, ti):
        nc = self.nc
        P = self.planning

        def colsrc(w, c0, n):
            return w[:, c0:c0 + n].rearrange("(kc p) j -> p kc j", p=128)
        self.norm_to_hT("nmix_col", l, router=False)
        if not P and ti == 0 and l == 0:
            self.build_diags()
        itA = self.item("win_conv%d" % l, 8, 1024, [(0, 8, 0, 1024, colsrc(self.w_in[l], 0, 1024))])
        for c in range(4):
            ba, bg = self.psum(), self.psum()
            if not P:
                self.mm_group(lambda ba=ba: self.PS[:, ba, :], self.PSB[ba], self.hT_specs(itA, lambda kc: kc, c * 128))
                self.mm_group(lambda bg=bg: self.PS[:, bg, :], self.PSB[bg], self.hT_specs(itA, lambda kc: kc, 512 + c * 128))
                a = self.arena(1)
                self.op("act", lambda a=a, bg=bg, c=c: nc.scalar.activation(
                    out=self.A32[:, a, :], in_=self.PS[:, bg, :], func=AF.Sigmoid, bias=self.bin_col[:, l, 4 + c:5 + c], scale=1.0),
                    reads=[self.PSB[bg], self.CONST], writes=[self.AB[a]])
                self.op("dve", lambda a=a, ba=ba, c=c: nc.vector.scalar_tensor_tensor(
                    out=self.HC[l][:, c, HALO_C:HALO_C + T], in0=self.PS[:, ba, :], scalar=self.bin_col[:, l, c:c + 1],
                    in1=self.A32[:, a, :], op0=ALU.add, op1=ALU.mult),
                    reads=[self.PSB[ba], self.AB[a], self.CONST], writes=[self.HCB[l][c]])
        rc, rq = [], []
        for c in range(4):
            itD = self.item("diag%d_%d" % (l, c), CK, 128, None)
            if P:
                continue
            bc = self.psum()
            specs = []
            for k in range(CK):
                specs.append(((lambda k=k, itD=itD: self.iap(itD, k, 0, 128)),
                              (lambda k=k, c=c: self.HC[l][:, c, k:k + T]),
                              [self.ibuf(itD), self.HCB[l][c], self.HCH[l][c]]))
            self.mm_group(lambda bc=bc: self.PS[:, bc, :], self.PSB[bc], specs)
            a = self.arena(2)
            rc.append(a)
            rq.append(a + 1)
            self.op("act", lambda a=a, bc=bc, c=c: nc.scalar.activation(
                out=self.A32[:, a, :], in_=self.PS[:, bc, :], func=AF.Identity, bias=self.convb_col[:, l, c:c + 1], scale=1.0),
                reads=[self.PSB[bc], self.CONST], writes=[self.AB[a]])
            self.op("act", lambda a=a, bc=bc, c=c: nc.scalar.activation(
                out=self.A32[:, a + 1, :], in_=self.PS[:, bc, :], func=AF.Square, bias=self.convb_col[:, l, c:c + 1], scale=1.0),
                reads=[self.PSB[bc], self.CONST], writes=[self.AB[a + 1]])
            self.op("act", lambda c=c: nc.scalar.copy(out=self.HC[l][:, c, 0:HALO_C], in_=self.HC[l][:, c, T:T + HALO_C]),
                    reads=[self.HCB[l][c]], writes=[self.HCH[l][c]])
        if not P:
            bm, bq = self.psum(), self.psum()
            self.mm_group(lambda: self.PS[:, bm, :], self.PSB[bm],
                          [((lambda: self.onesm[:]), (lambda c=c: self.A32[:, rc[c], :]), [self.AB[rc[c]], self.CONST]) for c in range(4)])
            self.mm_group(lambda: self.PS[:, bq, :], self.PSB[bq],
                          [((lambda: self.onesm[:]), (lambda c=c: self.A32[:, rq[c], :]), [self.AB[rq[c]], self.CONST]) for c in range(4)])
            am = self.arena(2)
            mean = lambda: self.A32[:, am, :]
            rstd = lambda: self.A32[:, am + 1, :]
            mb = [self.AB[am], self.AB[am + 1]]
            self.op("dve", lambda: nc.vector.tensor_copy(out=mean(), in_=self.PS[:, bm, :]), reads=[self.PSB[bm]], writes=[mb[0]])
            self.op("dve", lambda: nc.vector.tensor_tensor(out=rstd(), in0=mean(), in1=mean(), op=ALU.mult), reads=[mb[0]], writes=[mb[1]])
            self.op("dve", lambda: nc.vector.tensor_tensor(out=rstd(), in0=self.PS[:, bq, :], in1=rstd(), op=ALU.subtract),
                    reads=[self.PSB[bq], mb[1]], writes=[mb[1]])
            self.rsqrt_eps(rstd, [mb[1]])
            for c in range(4):
                self.op("dve", lambda c=c: nc.vector.tensor_tensor(out=self.A32[:, rc[c], :], in0=self.A32[:, rc[c], :], in1=mean(), op=ALU.subtract),
                        reads=[self.AB[rc[c]], mb[0]], writes=[self.AB[rc[c]]])
                self.op("dve", lambda c=c: nc.vector.tensor_tensor(out=self.A32[:, rc[c], :], in0=self.A32[:, rc[c], :], in1=rstd(), op=ALU.mult),
                        reads=[self.AB[rc[c]], mb[1]], writes=[self.AB[rc[c]]])
                self.op("act", lambda c=c: nc.scalar.activation(out=self.CT[:, c, :], in_=self.A32[:, rc[c], :], func=AF.Silu,
                                                                bias=self.clnb_col[:, l, c:c + 1], scale=self.clng_col[:, l, c:c + 1]),
                        reads=[self.AB[rc[c]], self.CONST], writes=[self.CTB[c]])
        itP = self.item("win_pool%d" % l, 8, 512, [(0, 8, 0, 512, colsrc(self.w_in[l], 1024, 512))])
        PPl = self.PP[l] if not P else None
        L = HALO_P + T
        if not P:
            for g, w in enumerate(POOL_W):
                pb = self.psum()
                self.mm_group(lambda pb=pb: self.PS[:, pb, :], self.PSB[pb], self.hT_specs(itP, lambda kc: kc, g * 128))
                self.op("act", lambda pb=pb, g=g: nc.scalar.activation(
                    out=PPl[:, g, HALO_P:HALO_P + T], in_=self.PS[:, pb, :], func=AF.Identity, bias=self.bin_col[:, l, 8 + g:9 + g], scale=1.0),
                    reads=[self.PSB[pb], self.CONST], writes=[self.PPB[l][g]])
        itU = self.item("win_u%d" % l, 8, 512, [(0, 8, 0, 512, colsrc(self.w_in[l], 1536, 512))])
        if not P:
            for h in range(4):
                pb = self.psum()
                self.mm_group(lambda pb=pb: self.PS[:, pb, :], self.PSB[pb], self.hT_specs(itU, lambda kc: kc, h * 128))
                self.gelu(lambda h=h: self.UT[:, h, :], lambda pb=pb: self.PS[:, pb, :], lambda h=h: self.bin_col[:, l, 12 + h:13 + h],
                          [self.PSB[pb], self.CONST], [self.UTB[h]])
            for g, w in enumerate(POOL_W):
                src_fn = (lambda g=g: PPl[:, g, :])
                src_b = [self.PPB[l][g], self.PPH[l][g]]
                tmp = [(self.PS1, self.PS1B), (self.PS2, self.PS2B)]
                sh = 1
                lvl = 0
                lo = 0
                while sh < w:
                    dstT, dstB = tmp[lvl % 2]
                    lo2 = lo + sh
                    self.op("dve", lambda src_fn=src_fn, dstT=dstT, lo2=lo2, sh=sh: nc.vector.tensor_tensor(
                        out=dstT[:, lo2:L], in0=src_fn()[:, lo2:L], in1=src_fn()[:, lo2 - sh:L - sh], op=ALU.add),
                        reads=src_b, writes=[dstB])
                    src_fn = (lambda dstT=dstT: dstT[:, 0:L])
                    src_b = [dstB]
                    lo = lo2
                    sh *= 2
                    lvl += 1
                assert lo == w - 1
                self.op("dve", lambda src_fn=src_fn, g=g, w=w: nc.vector.scalar_tensor_tensor(
                    out=self.DT[:, g, :], in0=src_fn()[:, HALO_P:L], scalar=1.0 / w, in1=PPl[:, g, HALO_P:L],
                    op0=ALU.mult, op1=ALU.subtract), reads=src_b + [self.PPB[l][g]], writes=[self.DTB[g]])
                if ti == 0:
                    a = self.arena(1)
                    self.op("dve", lambda src_fn=src_fn, g=g, a=a: nc.vector.tensor_tensor(
                        out=self.A32[:, a, 0:16], in0=src_fn()[:, HALO_P:HALO_P + 16], in1=self.icnt[:, g, :], op=ALU.mult),
                        reads=src_b + [self.CONST], writes=[self.AB[a]])
                    self.op("dve", lambda g=g, a=a: nc.vector.tensor_tensor(
                        out=self.DT[:, g, 0:16], in0=self.A32[:, a, 0:16], in1=PPl[:, g, HALO_P:HALO_P + 16], op=ALU.subtract),
                        reads=[self.AB[a], self.PPB[l][g]], writes=[self.DTB[g]])
                self.op("act", lambda g=g: nc.scalar.copy(out=PPl[:, g, 0:HALO_P], in_=PPl[:, g, T:T + HALO_P]),
                        reads=[self.PPB[l][g]], writes=[self.PPH[l][g]])
        itV = self.item("win_v%d" % l, 8, 512, [(0, 8, 0, 512, colsrc(self.w_in[l], 2048, 512))])
        if not P:
            vas = []
            for tb in range(4):
                pb = self.psum()
                specs = [((lambda kc=kc, tb=tb: self.HT[:, kc, tb * 128:(tb + 1) * 128]),
                          (lambda kc=kc: self.iap(itV, kc, 0, 512)), [self.ibuf(itV)] + self.HTB) for kc in range(8)]
                self.mm_group(lambda pb=pb: self.PS[:, pb, :], self.PSB[pb], specs)
                a = self.arena(1)
                vas.append(a)
                va = (lambda a=a: self.A32[:, a, :])
                vb = [self.AB[a]]
                self.op("dve", lambda pb=pb, va=va: nc.vector.tensor_tensor(out=va(), in0=self.PS[:, pb, :], in1=self.bv_bc[:, l, :], op=ALU.add),
                        reads=[self.PSB[pb], self.CONST], writes=vb)
                self.gelu(va, va, None, vb, vb)
            for g in range(4):
                pb2 = self.psum()
                self.mm_group(lambda pb2=pb2: self.PS[:, pb2, :], self.PSB[pb2],
                              [((lambda g=g: self.wmix[:, l, g, :]), (lambda g=g: self.DT[:, g, :]), [self.CONST, self.DTB[g]])])
                self.op("act", lambda pb2=pb2, g=g: nc.scalar.activation(
                    out=self.YP[:, g, :], in_=self.PS[:, pb2, :], func=AF.Copy, scale=self.pscale_col[:, l, g:g + 1]),
                    reads=[self.PSB[pb2], self.CONST], writes=[self.YPB[g]])
            sN = self.rstd4(lambda tb: [(lambda tb=tb: self.A32[:, vas[tb], :])], lambda tb: [self.AB[vas[tb]]], False)
            nb = self.SMB[sN]
            for tb in range(4):
                a = vas[tb]
                va = (lambda a=a: self.A32[:, a, :])
                vb = [self.AB[a]]
                self.op("dve", lambda va=va, tb=tb: nc.vector.tensor_scalar(
                    out=va(), in0=va(), scalar1=self.SM[:, sN, 2 * tb:2 * tb + 1], scalar2=self.SM[:, sN, 8 + tb:9 + tb],
                    op0=ALU.subtract, op1=ALU.mult), reads=vb + [nb], writes=vb)
                self.op("dve", lambda va=va: nc.vector.tensor_tensor(out=va(), in0=va(), in1=self.sgug_bc[:, l, :], op=ALU.mult),
                        reads=vb + [self.CONST], writes=vb)
                self.op("dve", lambda va=va, tb=tb: nc.vector.tensor_tensor(out=self.VL[:, tb, :], in0=va(), in1=self.sgub_bc[:, l, :], op=ALU.add),
                        reads=vb + [self.CONST], writes=[self.VLB[tb]])
            for h in range(4):
                pb = self.psum()
                self.op("pe", lambda pb=pb, h=h: nc.tensor.matmul(
                    self.PS[:, pb, :], self.onesrow[0:1, :], self.bsrow[0:1, l, h, :], start=True, stop=False),
                    reads=[self.CONST], writes=[self.PSB[pb]], signal=False)
                for tb in range(4):
                    self.op("pe", lambda pb=pb, tb=tb, h=h: nc.tensor.matmul(
                        self.PS[:, pb, tb * 128:(tb + 1) * 128], self.VL[:, tb, h * 128:(h + 1) * 128], self.wsT[:, l, h, :],
                        start=False, stop=(tb == 3)), reads=[self.VLB[tb], self.CONST], writes=[self.PSB[pb]], signal=(tb == 3))
                self.op("dve", lambda pb=pb, h=h: nc.vector.tensor_tensor(out=self.YS[:, h, :], in0=self.PS[:, pb, :], in1=self.UT[:, h, :], op=ALU.mult),
                        reads=[self.PSB[pb], self.UTB[h]], writes=[self.YSB[h]])
        outs_w = (self.conv_out[l], self.pool_out[l], self.sgu_out[l])
        for dc in range(8):
            itG = self.item("gate%d_%d" % (l, dc), 24, 128,
                            [(i * 8, 8, 0, 128, colsrc(self.w_gate[l], i * 1024 + dc * 128, 128)) for i in range(3)])
            if P:
                self.item("outs%d_%d" % (l, dc), 12, 128,
                          [(i * 4, 4, 0, 128, colsrc(outs_w[i], dc * 128, 128)) for i in range(3)])
                continue
            ga = []
            for i in range(3):
                pb = self.psum()
                self.mm_group(lambda pb=pb: self.PS[:, pb, :], self.PSB[pb], self.hT_specs(itG, lambda kc, i=i: i * 8 + kc, 0))
                a = self.arena(1)
                ga.append(a)
                self.op("act", lambda pb=pb, a=a, i=i: nc.scalar.activation(
                    out=self.A32[:, a, :], in_=self.PS[:, pb, :], func=AF.Sigmoid, bias=self.bgate_col[:, l, i * 8 + dc:i * 8 + dc + 1], scale=1.0),
                    reads=[self.PSB[pb], self.CONST], writes=[self.AB[a]])
            itO = self.item("outs%d_%d" % (l, dc))
            srcs = ((self.CT, self.CTB), (self.YP, self.YPB), (self.YS, self.YSB))
            for i in range(3):
                pb = self.psum()
                sT, sB = srcs[i]
                specs = [((lambda c=c, i=i: self.iap(itO, i * 4 + c, 0, 128)), (lambda c=c, sT=sT: sT[:, c, :]), [self.ibuf(itO), sB[c]])
                         for c in range(4)]
                self.mm_group(lambda pb=pb: self.PS[:, pb, :], self.PSB[pb], specs)
                a = ga[i]
                self.op("dve", lambda pb=pb, a=a: nc.vector.tensor_tensor(out=self.A32[:, a, :], in0=self.A32[:, a, :], in1=self.PS[:, pb, :], op=ALU.mult),
                        reads=[self.PSB[pb], self.AB[a]], writes=[self.AB[a]])
            self.op("dve", lambda ga=ga: nc.vector.tensor_tensor(out=self.A32[:, ga[0], :], in0=self.A32[:, ga[0], :], in1=self.A32[:, ga[1], :], op=ALU.add),
                    reads=[self.AB[ga[0]], self.AB[ga[1]]], writes=[self.AB[ga[0]]])
            self.op("dve", lambda ga=ga, dc=dc: nc.vector.tensor_tensor(out=self.HID[:, 0, dc, :], in0=self.A32[:, ga[0], :], in1=self.A32[:, ga[2], :], op=ALU.add),
                    reads=[self.AB[ga[0]], self.AB[ga[2]]], writes=[self.HIDB[0][dc]])
        for dh in range(2):
            itW = self.item("wo%d_%d" % (l, dh), 8, 512, [(0, 8, 0, 512, colsrc(self.w_o[l], dh * 512, 512))])
            if P:
                continue
            for tb in range(4):
                pb = self.psum()
                specs = [((lambda kc=kc, tb=tb: self.HID[:, 0, kc, tb * 128:(tb + 1) * 128]),
                          (lambda kc=kc, itW=itW: self.iap(itW, kc, 0, 512)), [self.ibuf(itW), self.HIDB[0][kc]]) for kc in range(8)]
                self.mm_group(lambda pb=pb: self.PS[:, pb, :], self.PSB[pb], specs)
                self.op("dve", lambda pb=pb, tb=tb, dh=dh: nc.vector.tensor_tensor(
                    out=self.X[:, tb, dh, :], in0=self.X[:, tb, dh, :], in1=self.PS[:, pb, :], op=ALU.add),
                    reads=[self.PSB[pb], self.XB[tb][dh]], writes=[self.XB[tb][dh]])

    def ffn(self, pfx, nf, w_up, w_down, F, expert):
        nc = self.nc
        P = self.planning
        segs = []
        f = 0
        while f < nf:
            n = min(8, nf - f)
            segs.append((f, n))
            f += n
        ups = {}

        def up_item(q):
            nq = min(4, nf - q * 4)
            pieces = [(0, 8, 0, nq * 128, w_up[:, q * 512:q * 512 + nq * 128].rearrange("(kc p) j -> p kc j", p=128)),
                      (0, 8, nq * 128, nq * 128, w_up[:, F + q * 512:F + q * 512 + nq * 128].rearrange("(kc p) j -> p kc j", p=128))]
            return self.item("%s_up%d" % (pfx, q), 8, 2 * nq * 128, pieces)

        def do_up(si):
            f0, n = segs[si]
            hb = si % 2
            for jj in range(n):
                fch = f0 + jj
                q = fch // 4
                if (fch % 4) == 0:
                    ups[q] = up_item(q)
                if P:
                    continue
                it = ups[q]
                j4 = fch % 4
                ba, bb = self.psum(), self.psum()
                self.mm_group(lambda ba=ba: self.PS[:, ba, :], self.PSB[ba], self.hT_specs(it, lambda kc: kc, j4 * 128))
                self.mm_group(lambda bb=bb: self.PS[:, bb, :], self.PSB[bb], self.hT_specs(it, lambda kc: kc, it.W // 2 + j4 * 128))
                a = self.arena(1)
                self.op("act", lambda a=a, ba=ba: nc.scalar.activation(out=self.A32[:, a, :], in_=self.PS[:, ba, :], func=AF.Silu),
                        reads=[self.PSB[ba]], writes=[self.AB[a]])
                self.op("dve", lambda a=a, bb=bb, hb=hb, jj=jj: nc.vector.tensor_tensor(
                    out=self.HID[:, hb, jj, :], in0=self.A32[:, a, :], in1=self.PS[:, bb, :], op=ALU.mult),
                    reads=[self.AB[a], self.PSB[bb]], writes=[self.HIDB[hb][jj]])

        def do_down(si):
            f0, n = segs[si]
            hb = si % 2
            it = self.item("%s_down%d" % (pfx, si), n, 1024,
                           [(0, n, 0, 1024, w_down[f0 * 128:(f0 + n) * 128, :].rearrange("(r p) j -> p r j", p=128))])
            if P:
                return
            for tb in range(4):
                for dh in range(2):
                    pb = self.psum()
                    specs = [((lambda jj=jj, tb=tb: self.HID[:, hb, jj, tb * 128:(tb + 1) * 128]),
                              (lambda jj=jj, dh=dh: self.iap(it, jj, dh * 512, 512)), [self.ibuf(it), self.HIDB[hb][jj]]) for jj in range(n)]
                    self.mm_group(lambda pb=pb: self.PS[:, pb, :], self.PSB[pb], specs)
                    if expert is None:
                        self.op("dve", lambda pb=pb, tb=tb, dh=dh: nc.vector.tensor_tensor(
                            out=self.X[:, tb, dh, :], in0=self.X[:, tb, dh, :], in1=self.PS[:, pb, :], op=ALU.add),
                            reads=[self.PSB[pb], self.XB[tb][dh]], writes=[self.XB[tb][dh]])
                    else:
                        self.op("dve", lambda pb=pb, tb=tb, dh=dh: nc.vector.scalar_tensor_tensor(
                            out=self.X[:, tb, dh, :], in0=self.PS[:, pb, :], scalar=self.GATE[:, tb, expert:expert + 1],
                            in1=self.X[:, tb, dh, :], op0=ALU.mult, op1=ALU.add),
                            reads=[self.PSB[pb], self.XB[tb][dh], self.GATEB[tb]], writes=[self.XB[tb][dh]])
        do_up(0)
        for si in range(len(segs)):
            if si + 1 < len(segs):
                do_up(si + 1)
            do_down(si)

    def final_norm(self, ti, plain=False):
        nc = self.nc
        if self.planning:
            return
        if not plain:
            sN = self.rstd4(lambda tb: [(lambda tb=tb, dh=dh: self.X[:, tb, dh, :]) for dh in range(2)], lambda tb: self.XB[tb], True)
            nb = self.SMB[sN]
        for tb in range(4):
            a = self.arena(2)
            ab = [self.AB[a], self.AB[a + 1]]
            if plain:
                self.op("dve", lambda a=a, tb=tb: nc.vector.tensor_copy(out=self.A32[:, a:a + 2, :], in_=self.X[:, tb, :, :]),
                        reads=self.XB[tb], writes=ab)
            else:
                self.op("dve", lambda a=a, tb=tb: nc.vector.scalar_tensor_tensor(
                    out=self.A32[:, a:a + 2, :], in0=self.X[:, tb, :, :], scalar=self.SM[:, sN, 8 + tb:9 + tb], in1=self.nfin_bc[:],
                    op0=ALU.mult, op1=ALU.mult), reads=self.XB[tb] + [nb, self.CONST], writes=ab)
            r0 = ti * T + tb * 128
            self.dma("act", self.out_d[r0:r0 + 128, :].rearrange("p (a b) -> p a b", a=2), self.A32[:, a:a + 2, :],
                     "out%d" % tb, reads=ab)


_CACHE = {}
WNAMES = ["norm_mix", "w_in", "b_in", "conv_w", "conv_b", "conv_ln_g", "conv_ln_b", "conv_out", "pool_mix", "pool_scale",
          "pool_out", "sgu_ln_g", "sgu_ln_b", "sgu_w", "sgu_b", "sgu_out", "w_gate", "b_gate", "w_o", "norm_ffn",
          "ffn_w_up", "ffn_w_down", "moe_router", "moe_w_up", "moe_w_down", "norm_final"]


def run(inputs, nt, ncores, nexp=NE, upto="all", trace=False):
    key = (nt, nexp, upto)
    if key not in _CACHE:
        _CACHE[key] = Builder(nt, nt * T, nexp, upto).build()
    nc = _CACHE[key]
    x = np.asarray(inputs["x"], dtype=np.float32)
    shared = {k: np.ascontiguousarray(np.asarray(inputs[k], dtype=np.float32)) for k in WNAMES}
    in_maps = []
    for b in range(ncores):
        m = dict(shared)
        m["x"] = np.ascontiguousarray(x[b, :nt * T])
        in_maps.append(m)
    res = run_bass_kernel_spmd(nc, in_maps, core_ids=list(range(ncores)), trace=trace)
    out = np.stack([np.asarray(r["out"]) for r in res.results], axis=0)
    return out, res


def kernel(**inputs):
    out, _ = run(inputs, nt=8, ncores=8)
    return out.astype(np.float32)
```
